# Optimizing a Trainium2 kernel written in Bass

```python
import jax, jax.numpy as jnp
from jax import lax
import numpy as np

D_MODEL = 1024
BATCH = 8
SEQ = 2048
DEPTH = 1

CHUNK = 64
Q_BLOCK = 128
MLA_HEADS = 8
MLA_NOPE = 64
MLA_ROPE = 32
MLA_V = 64
Q_LORA = 384
KV_LORA = 256
ROPE_THETA = 10000.0
HG_HEADS = 4
HG_DK = 128
HG_DV = 128
D_MIX = MLA_HEADS * MLA_V + HG_HEADS * HG_DV
IN_SPLITS = (Q_LORA, KV_LORA, MLA_ROPE, HG_HEADS * HG_DK, HG_HEADS * HG_DK, HG_HEADS * HG_DV, HG_HEADS * HG_DV)
IN_COLS = Q_LORA + KV_LORA + MLA_ROPE + 2 * HG_HEADS * HG_DK + 2 * HG_HEADS * HG_DV
N_EXPERTS = 32
TOP_K = 4
D_EXPERT = 1024
SWIGLU_LIMIT = 7.0
SWIGLU_ALPHA = 1.702
MOE_BLOCK = 128
LN_EPS = 1e-5
RMS_EPS = 1e-6
DEEPNORM_ALPHA = (2.0 * DEPTH) ** 0.25
DEEPNORM_BETA = (8.0 * DEPTH) ** -0.25

kernel_name = "hymba_mla_hgrn2_gptoss_moe_deepnorm"


def layer_norm(x, g, b):
    xf = x.astype(jnp.float32)
    mu = jnp.mean(xf, axis=-1, keepdims=True)
    var = jnp.mean(jnp.square(xf - mu), axis=-1, keepdims=True)
    return ((xf - mu) * lax.rsqrt(var + LN_EPS) * g + b).astype(x.dtype)


def rms_norm(x, g):
    xf = x.astype(jnp.float32)
    return (xf * lax.rsqrt(jnp.mean(jnp.square(xf), axis=-1, keepdims=True) + RMS_EPS) * g).astype(x.dtype)


def rope_cos_sin(positions):
    inv_freq = ROPE_THETA ** (-jnp.arange(0, MLA_ROPE, 2, dtype=jnp.float32) / MLA_ROPE)
    ang = positions.astype(jnp.float32)[..., None] * inv_freq
    return jnp.cos(ang), jnp.sin(ang)


def apply_rope(x, cos, sin):
    x1, x2 = jnp.split(x.astype(jnp.float32), 2, axis=-1)
    return jnp.concatenate([x1 * cos - x2 * sin, x1 * sin + x2 * cos], axis=-1).astype(x.dtype)


def mla_attention(q_nope, q_rope, k_nope, k_rope, v):
    B, S, H, _ = q_nope.shape
    n_blocks = S // Q_BLOCK
    key_chunk = jnp.arange(S) // CHUNK
    scale = (MLA_NOPE + MLA_ROPE) ** -0.5

    def one_block(i):
        start = i * Q_BLOCK
        qn = lax.dynamic_slice_in_dim(q_nope, start, Q_BLOCK, axis=1)
        qr = lax.dynamic_slice_in_dim(q_rope, start, Q_BLOCK, axis=1)
        s = (jnp.einsum("bqhd,bkhd->bhqk", qn, k_nope)
             + jnp.einsum("bqhr,bkr->bhqk", qr, k_rope)).astype(jnp.float32) * scale
        q_chunk = (start + jnp.arange(Q_BLOCK)) // CHUNK
        mask = key_chunk[None, :] <= q_chunk[:, None]
        p = jax.nn.softmax(jnp.where(mask, s, -jnp.inf), axis=-1).astype(v.dtype)
        return jnp.einsum("bhqk,bkhd->bqhd", p, v)

    out = lax.map(one_block, jnp.arange(n_blocks))
    return out.transpose(1, 0, 2, 3, 4).reshape(B, S, H * v.shape[-1])


def hgrn2_chunkwise(q, k, log_f, v):
    B, S, H, DK = q.shape
    DV = v.shape[-1]
    N = S // CHUNK

    def to_chunks(a):
        return a.astype(jnp.float32).reshape(B, N, CHUNK, H, a.shape[-1]).swapaxes(0, 1)

    causal = jnp.tril(jnp.ones((CHUNK, CHUNK), dtype=bool))[None, :, :, None, None]

    def step(state, inp):
        qc, kc, lfc, vc = inp
        b = jnp.cumsum(lfc, axis=1)
        b_last = b[:, -1]
        diff = b[:, :, None] - b[:, None, :]
        decay = jnp.exp(jnp.where(causal, diff, -jnp.inf))
        scores = jnp.einsum("bthk,btshk,bshk->bhts", qc, decay, kc)
        o = (jnp.einsum("bhts,bshv->bthv", scores, vc)
             + jnp.einsum("bthk,bhkv->bthv", qc * jnp.exp(b), state))
        new_state = (jnp.exp(b_last)[..., None] * state
                     + jnp.einsum("bshk,bshv->bhkv", kc * jnp.exp(b_last[:, None] - b), vc))
        return new_state, o

    s0 = jnp.zeros((B, H, DK, DV), jnp.float32)
    _, o = lax.scan(step, s0, (to_chunks(q), to_chunks(k), to_chunks(log_f), to_chunks(v)))
    return o.swapaxes(0, 1).reshape(B, S, H, DV)


def hybrid_mixer(h, cos, sin, lb, w_in, q_a_norm_g, w_q_b, kv_a_norm_g, w_kv_b, mla_out_g, hgrn_out_g, w_o):
    B, S, _ = h.shape
    offsets = [int(o) for o in np.cumsum(IN_SPLITS)[:-1]]
    q_a, kv_a, k_rope, hq, hf, hi, hg = jnp.split(h @ w_in, offsets, axis=-1)

    q = (rms_norm(q_a, q_a_norm_g) @ w_q_b).reshape(B, S, MLA_HEADS, MLA_NOPE + MLA_ROPE)
    q_nope = q[..., :MLA_NOPE]
    q_rope = apply_rope(q[..., MLA_NOPE:], cos[:, :, None], sin[:, :, None])
    kv = (rms_norm(kv_a, kv_a_norm_g) @ w_kv_b).reshape(B, S, MLA_HEADS, MLA_NOPE + MLA_V)
    k_nope, v = kv[..., :MLA_NOPE], kv[..., MLA_NOPE:]
    k_rope = apply_rope(k_rope, cos, sin)
    attn = rms_norm(mla_attention(q_nope, q_rope, k_nope, k_rope, v), mla_out_g)

    def heads(a, d):
        return a.reshape(B, S, HG_HEADS, d)
    forget = lb + (1.0 - lb) * jax.nn.sigmoid(hf.astype(jnp.float32))
    o = hgrn2_chunkwise(heads(jax.nn.silu(hq), HG_DK), heads(1.0 - forget, HG_DK),
                        heads(jnp.log(forget), HG_DK), heads(hi, HG_DV))
    o = rms_norm(o, hgrn_out_g.reshape(HG_HEADS, HG_DV)) * jax.nn.silu(heads(hg, HG_DV).astype(jnp.float32))
    o = o.reshape(B, S, HG_HEADS * HG_DV).astype(h.dtype)

    return jnp.concatenate([attn, o], axis=-1) @ w_o


def moe_ffn(h, w_router, b_router, w_up, b_up, w_down, b_down):
    B, S, D = h.shape
    T = B * S
    A = T * TOP_K
    xt = h.reshape(T, D)
    logits = (xt @ w_router + b_router).astype(jnp.float32)
    top_logit, top_e = lax.top_k(logits, TOP_K)
    gate = jax.nn.softmax(top_logit, axis=-1)

    flat_e = top_e.reshape(A)
    order = jnp.argsort(flat_e, stable=True)
    sorted_e = flat_e[order]
    sorted_tok = (order // TOP_K).astype(jnp.int32)
    sorted_gate = gate.reshape(A)[order]
    counts = jnp.bincount(flat_e, length=N_EXPERTS)
    padded = (counts + MOE_BLOCK - 1) // MOE_BLOCK * MOE_BLOCK
    start = jnp.cumsum(counts) - counts
    pad_end = jnp.cumsum(padded)
    pad_start = pad_end - padded
    dest = pad_start[sorted_e] + (jnp.arange(A) - start[sorted_e])
    n_blocks = -(-A // MOE_BLOCK) + N_EXPERTS
    P = n_blocks * MOE_BLOCK
    slot_tok = jnp.zeros((P,), jnp.int32).at[dest].set(sorted_tok)
    slot_gate = jnp.zeros((P,), jnp.float32).at[dest].set(sorted_gate)
    block_e = jnp.minimum(jnp.searchsorted(pad_end, jnp.arange(n_blocks) * MOE_BLOCK, side="right"),
                          N_EXPERTS - 1)
    xs = xt[slot_tok].reshape(n_blocks, MOE_BLOCK, D)

    def expert_block(args):
        xb, e = args
        hb = xb @ w_up[e] + b_up[e]
        glu, lin = jnp.split(hb, 2, axis=-1)
        glu = jnp.minimum(glu, SWIGLU_LIMIT)
        lin = jnp.clip(lin, -SWIGLU_LIMIT, SWIGLU_LIMIT)
        act = glu * jax.nn.sigmoid(SWIGLU_ALPHA * glu) * (lin + 1.0)
        return act @ w_down[e] + b_down[e]

    ys = lax.map(expert_block, (xs, block_e)).reshape(P, D)
    ys = ys * slot_gate[:, None].astype(ys.dtype)
    out = jnp.zeros((T, D), h.dtype).at[slot_tok].add(ys.astype(h.dtype))
    return out.reshape(B, S, D)


def setup_inputs(seed: int = 0) -> dict:
    key = jax.random.key(seed)
    ks = jax.random.split(key, 24)
    f32 = jnp.float32

    def dense(k, shape, fan_in, scale=1.0):
        return jax.random.normal(k, shape, f32) * (scale * fan_in ** -0.5)

    def gain(k, shape):
        return 1.0 + 0.02 * jax.random.normal(k, shape, f32)

    def bias(k, shape, s=0.02):
        return s * jax.random.normal(k, shape, f32)

    x = jax.random.normal(ks[0], (BATCH, SEQ, D_MODEL), f32)
    positions = (jax.random.randint(ks[1], (BATCH, 1), 0, 4096, dtype=jnp.int32)
                 + jnp.arange(SEQ, dtype=jnp.int32)[None, :])
    hv = HG_HEADS * HG_DV
    in_col_scale = jnp.concatenate([jnp.ones((IN_COLS - 2 * hv,), f32),
                                    jnp.full((hv,), DEEPNORM_BETA, f32),
                                    jnp.ones((hv,), f32)])
    kvb_col_scale = jnp.tile(jnp.concatenate([jnp.ones((MLA_NOPE,), f32),
                                              jnp.full((MLA_V,), DEEPNORM_BETA, f32)]), MLA_HEADS)
    return {
        "x": x,
        "positions": positions,
        "ln_in_g": gain(ks[2], (D_MODEL,)),
        "ln_in_b": bias(ks[3], (D_MODEL,)),
        "w_in": dense(ks[4], (DEPTH, D_MODEL, IN_COLS), D_MODEL) * in_col_scale,
        "q_a_norm_g": gain(ks[5], (DEPTH, Q_LORA)),
        "w_q_b": dense(ks[6], (DEPTH, Q_LORA, MLA_HEADS * (MLA_NOPE + MLA_ROPE)), Q_LORA),
        "kv_a_norm_g": gain(ks[7], (DEPTH, KV_LORA)),
        "w_kv_b": dense(ks[8], (DEPTH, KV_LORA, MLA_HEADS * (MLA_NOPE + MLA_V)), KV_LORA) * kvb_col_scale,
        "hgrn_lb_logits": 0.5 * jax.random.normal(ks[9], (DEPTH + 1, HG_HEADS * HG_DK), f32),
        "mla_out_g": gain(ks[10], (DEPTH, MLA_HEADS * MLA_V)),
        "hgrn_out_g": gain(ks[11], (DEPTH, HG_HEADS * HG_DV)),
        "w_o": dense(ks[12], (DEPTH, D_MIX, D_MODEL), D_MIX, DEEPNORM_BETA),
        "ln1_g": gain(ks[13], (DEPTH, D_MODEL)),
        "ln1_b": bias(ks[14], (DEPTH, D_MODEL)),
        "w_router": dense(ks[15], (DEPTH, D_MODEL, N_EXPERTS), D_MODEL),
        "b_router": bias(ks[16], (DEPTH, N_EXPERTS), 0.01),
        "w_up": dense(ks[17], (DEPTH, N_EXPERTS, D_MODEL, 2 * D_EXPERT), D_MODEL),
        "b_up": bias(ks[18], (DEPTH, N_EXPERTS, 2 * D_EXPERT)),
        "w_down": dense(ks[19], (DEPTH, N_EXPERTS, D_EXPERT, D_MODEL), D_EXPERT, DEEPNORM_BETA),
        "b_down": bias(ks[20], (DEPTH, N_EXPERTS, D_MODEL)),
        "ln2_g": gain(ks[21], (DEPTH, D_MODEL)),
        "ln2_b": bias(ks[22], (DEPTH, D_MODEL)),
    }


def reference(x, positions, ln_in_g, ln_in_b, w_in, q_a_norm_g, w_q_b, kv_a_norm_g, w_kv_b,
              hgrn_lb_logits, mla_out_g, hgrn_out_g, w_o, ln1_g, ln1_b, w_router, b_router,
              w_up, b_up, w_down, b_down, ln2_g, ln2_b):
    cos, sin = rope_cos_sin(positions)
    lower_bounds = jnp.cumsum(jax.nn.softmax(hgrn_lb_logits.astype(jnp.float32), axis=0), axis=0)
    h = layer_norm(x, ln_in_g, ln_in_b)
    for l in range(DEPTH):
        mix = hybrid_mixer(h, cos, sin, lower_bounds[l], w_in[l], q_a_norm_g[l], w_q_b[l],
                           kv_a_norm_g[l], w_kv_b[l], mla_out_g[l], hgrn_out_g[l], w_o[l])
        h = layer_norm(DEEPNORM_ALPHA * h + mix, ln1_g[l], ln1_b[l])
        ffn = moe_ffn(h, w_router[l], b_router[l], w_up[l], b_up[l], w_down[l], b_down[l])
        h = layer_norm(DEEPNORM_ALPHA * h + ffn, ln2_g[l], ln2_b[l])
    return h
```

```python
import numpy as np
import concourse.bass as bass
import concourse.mybir as mybir
from concourse.bass_utils import run_bass_kernel_spmd

F32 = mybir.dt.float32
BF16 = mybir.dt.bfloat16
I32 = mybir.dt.int32
AF = mybir.ActivationFunctionType
ALU = mybir.AluOpType

S = 2048
D = 1024
NT = 16
INC = 2720
NE = 32
ALPHA = 2.0 ** 0.25
TWO_PI = 6.283185307179586
C1 = 6.28125
C2 = TWO_PI - C1
PI = 3.141592653589793


class Sched:
    ENGS = ("pe", "act", "dve", "pool", "sp")

    def __init__(self):
        self.ops = []
        self.last_writer = {}
        self.readers = {}
        self.dma_count = {}
        self.last_dma_on_sem = {}
        self.pending_barrier = {}

    def add(self, eng, fn, reads=(), writes=(), dma_sem=None):
        i = len(self.ops)
        deps = set()
        writes = list(writes) + [k for k in reads if isinstance(k, tuple) and k and k[0] == "ps" and k not in writes]
        for k in reads:
            w = self.last_writer.get(k)
            if w is not None:
                deps.add(w)
        for k in writes:
            w = self.last_writer.get(k)
            if w is not None:
                deps.add(w)
            deps.update(self.readers.get(k, ()))
        for k in reads:
            self.readers.setdefault(k, []).append(i)
        for k in writes:
            self.last_writer[k] = i
            self.readers[k] = []
        if eng in self.pending_barrier:
            deps.update(self.pending_barrier.pop(eng))
        op = dict(eng=eng, fn=fn, deps=deps, dma_sem=dma_sem, dma_val=None)
        if dma_sem is not None:
            prev = self.last_dma_on_sem.get(dma_sem)
            if prev is not None:
                deps.add(prev)
            self.last_dma_on_sem[dma_sem] = i
            c = self.dma_count.get(dma_sem, 0) + 1
            self.dma_count[dma_sem] = c
            op["dma_val"] = 16 * c
        self.ops.append(op)
        return i

    def barrier(self):
        last = {}
        dmas = []
        for i, op in enumerate(self.ops):
            if op["dma_sem"] is not None:
                dmas.append(i)
            else:
                last[op["eng"]] = i
        deps = set(last.values()) | set(self.last_dma_on_sem.values())
        for e in self.ENGS:
            self.pending_barrier[e] = set(deps)

    def emit(self, block_engines, sems, dma_sems):
        ops = self.ops
        n = len(ops)
        has_consumer = [False] * n
        for i, op in enumerate(ops):
            keep = set()
            for d in op["deps"]:
                dop = ops[d]
                if (dop["dma_sem"] is None and op["dma_sem"] is None
                        and dop["eng"] == "pe" and op["eng"] == "pe"):
                    continue
                keep.add(d)
                has_consumer[d] = True
            op["deps"] = keep
        sig = {e: 0 for e in self.ENGS}
        for i, op in enumerate(ops):
            if op["dma_sem"] is None and has_consumer[i]:
                assert op["fn"] is not None
                sig[op["eng"]] += 1
                op["sig"] = sig[op["eng"]]
            else:
                op["sig"] = None
        per_eng = {e: [] for e in self.ENGS}
        for i, op in enumerate(ops):
            per_eng[op["eng"]].append(i)

        def make_body(e):
            def body(eng):
                waited = {}
                for i in per_eng[e]:
                    op = ops[i]
                    need = {}
                    for d in op["deps"]:
                        dop = ops[d]
                        if dop["dma_sem"] is not None:
                            key = ("d", dop["dma_sem"])
                            val = dop["dma_val"]
                        else:
                            key = ("e", dop["eng"])
                            val = dop["sig"]
                        if val > need.get(key, 0):
                            need[key] = val
                    for key, val in need.items():
                        if val > waited.get(key, 0):
                            waited[key] = val
                            s = dma_sems[key[1]] if key[0] == "d" else sems[key[1]]
                            eng.wait_ge(s, val)
                    if op["fn"] is None:
                        continue
                    ins = op["fn"](eng)
                    if op["dma_sem"] is not None:
                        ins.then_inc(dma_sems[op["dma_sem"]], 16)
                    elif op["sig"] is not None:
                        ins.then_inc(sems[e], 1)
            return body

        for e in self.ENGS:
            if per_eng[e]:
                block_engines[e](make_body(e))


SB_BASE = 16512
SB_TOP = 229344
_cnt = [0]


class Arena:
    def __init__(self, nc, start=SB_BASE):
        self.nc = nc
        self.base = start
        self.top = SB_TOP
        self.cur = start
        self.peak = 0

    def alloc(self, name, shape, dt):
        esz = 2 if dt == BF16 else 4
        size = esz
        for s in shape[1:]:
            size *= s
        size = (size + 31) // 32 * 32
        assert self.cur + size <= self.top, f"SBUF overflow allocating {name}: {self.cur + size - self.base}"
        t = self.nc.alloc_sbuf_tensor_at(f"{name}_{_cnt[0]}", list(shape), dt, offset=self.cur)
        _cnt[0] += 1
        self.cur += size
        self.peak = max(self.peak, self.cur - self.base)
        return t

    def mark(self):
        return self.cur

    def release(self, m):
        self.cur = m


def host_consts():
    c = np.zeros((128, 784), np.float32)
    c[:, 0:128] = np.eye(128, dtype=np.float32)
    s = np.arange(128)[:, None]
    t = np.arange(128)[None, :]
    c[:, 128:256] = ((s // 64 == t // 64) & (s <= t)).astype(np.float32)
    inv_freq = (10000.0 ** (-np.arange(0, 32, 2, dtype=np.float32) / np.float32(32))).astype(np.float32)
    c[96:112, 256] = inv_freq
    c[112:128, 256] = inv_freq
    c[:, 272:400] = (s < t).astype(np.float32)
    c[:, 400:784] = np.arange(384, dtype=np.float32)[None, :]
    return c


def build_program(debug=(), n_experts=NE, stop_after=None):
    nc = bass.Bass("TRN2", target_bir_lowering=False)
    dr = {}

    def din(name, shape, dt=F32):
        dr[name] = nc.dram_tensor(name, list(shape), dt, kind="ExternalInput").ap()
        return dr[name]

    x = din("x", [S, D])
    pos = din("pos", [1, S], I32)
    cst = din("cst", [128, 784])
    ln_in_g = din("ln_in_g", [1, D]); ln_in_b = din("ln_in_b", [1, D])
    w_in = din("w_in", [D, INC])
    q_a_norm_g = din("q_a_norm_g", [3, 128])
    w_q_b = din("w_q_b", [384, 768])
    kv_a_norm_g = din("kv_a_norm_g", [2, 128])
    w_kv_b = din("w_kv_b", [256, 1024])
    lb_logits = din("hgrn_lb_logits", [8, 128])
    mla_out_g = din("mla_out_g", [4, 128])
    hgrn_out_g = din("hgrn_out_g", [4, 128])
    w_o = din("w_o", [D, D])
    ln1_g = din("ln1_g", [1, D]); ln1_b = din("ln1_b", [1, D])
    w_router = din("w_router", [D, NE])
    b_router = din("b_router", [1, NE])
    w_up = din("w_up", [NE, D, 2048])
    b_up = din("b_up", [NE * 16, 128])
    w_down = din("w_down", [NE, D, D])
    b_down = din("b_down", [NE, D])
    ln2_g = din("ln2_g", [1, D]); ln2_b = din("ln2_b", [1, D])
    out = nc.dram_tensor("out", [S, D], F32, kind="ExternalOutput").ap()
    h1d = nc.dram_tensor("h1_scratch", [S, D], F32).ap()

    sc = Sched()
    ar = Arena(nc)
    ps = nc.alloc_psum_tensor("ps", [128, 8, 512], F32)

    dma_names = []

    def A(eng, fn, r=(), w=(), dma=None):
        if dma is not None and dma not in dma_names:
            dma_names.append(dma)
        return sc.add(eng, fn, reads=r, writes=w, dma_sem=dma)

    taps = {}

    def tap(name, ap_fn, shape, dt, reads):
        if name not in debug:
            return
        t = nc.dram_tensor("dbg_" + name, list(shape), dt, kind="ExternalOutput").ap()
        taps[name] = t
        A("sp", lambda e: e.dma_start(out=t, in_=ap_fn()), r=reads, w=[("tapout", name)], dma="tap_" + name)

    def psb(b):
        return ps[:, b, :]

    def psb16(b):
        return ps[:, b, :].bitcast(BF16)

    cs = ar.alloc("cs", [128, 784], F32)
    identf = cs[:, 0:128]
    m2 = cs[:, 128:256]
    invf = cs[:, 256:257]
    identb = ar.alloc("identb", [128, 128], BF16)
    onesb = ar.alloc("onesb", [128, 128], BF16)
    oz = ar.alloc("oz", [128, 192], BF16)
    pst = ar.alloc("pst", [128, 128], F32)
    pcol = ar.alloc("pcol", [128, 32], F32)
    lbc = ar.alloc("lbc", [128, 4], F32)
    omlb = ar.alloc("omlb", [128, 4], F32)
    stat_rs = ar.alloc("stat_rs", [128, NT], F32)
    stat_nm = ar.alloc("stat_nm", [128, NT], F32)
    MIX_AT = ar.cur
    mixT = ar.alloc("mixT", [128, 8, S], BF16)
    P0_END = ar.cur
    Wq = ar.alloc("Wq", [128, 3, 8, 128], BF16)
    Wq2 = ar.alloc("Wq2", [128, 3, 8, 128], BF16)
    Wk = ar.alloc("Wk", [128, 2, 8, 64], BF16)
    Wv = ar.alloc("Wv", [128, 2, 8, 64], BF16)
    CW_END = ar.cur

    A("sp", lambda e: e.dma_start(out=cs[:], in_=cst), w=["cs"], dma="c0")
    A("dve", lambda e: e.tensor_copy(out=identb[:], in_=identf), r=["cs"], w=["identb"])
    A("dve", lambda e: e.memset(onesb[:], 1.0), w=["onesb"])
    A("dve", lambda e: e.memset(oz[:], 0.0), w=["oz"])
    A("dve", lambda e: e.memset(oz[:, 64:128], 1.0), w=["oz"])
    A("dve", lambda e: e.memset(pst[:], 0.0), w=["pst"])
    A("sp", lambda e: e.dma_start(out=pst[0:3, :], in_=q_a_norm_g), w=["pst"], dma="c1")
    A("sp", lambda e: e.dma_start(out=pst[3:5, :], in_=kv_a_norm_g), w=["pst"], dma="c1")
    A("sp", lambda e: e.dma_start(out=pst[5:9, :], in_=mla_out_g), w=["pst"], dma="c1")
    A("sp", lambda e: e.dma_start(out=pst[9:13, :], in_=hgrn_out_g), w=["pst"], dma="c1")
    A("sp", lambda e: e.dma_start(out=pst[13:21, :], in_=lb_logits), w=["pst"], dma="c1")
    A("pe", lambda e: e.transpose(out=ps[:, 0, 0:128], in_=pst[:], identity=identf), r=["pst", "cs"], w=[("ps", 0)])
    A("dve", lambda e: e.tensor_copy(out=pcol[:], in_=ps[:, 0, 0:32]), r=[("ps", 0)], w=["pcol"])
    A("dve", lambda e: e.tensor_tensor(out=omlb[:], in0=pcol[:, 13:17], in1=pcol[:, 17:21], op=ALU.subtract), r=["pcol"], w=["omlb"])
    A("act", lambda e: e.activation(out=lbc[:], in_=omlb[:], func=AF.Sigmoid), r=["omlb"], w=["lbc"])
    A("dve", lambda e: e.tensor_scalar(out=omlb[:], in0=lbc[:], scalar1=-1.0, scalar2=1.0, op0=ALU.mult, op1=ALU.add), r=["lbc"], w=["omlb"])
    QG, KVG, MOG, HOG = 0, 3, 5, 9

    def body():
        nonlocal ar
        hT = ar.alloc("hT", [128, 8, S], BF16)
        Win = ar.alloc("Win", [128, 8, INC], BF16)
        Wkr2 = ar.alloc("Wkr2", [128, 8, 128], BF16)
        Y_START = ar.cur
        ar = Arena(nc, Y_START)
        Gb = ar.alloc("Gb", [128, D], F32)
        Bb = ar.alloc("Bb", [128, D], F32)
        A("sp", lambda e: e.dma_start(out=Gb[:], in_=ln_in_g.partition_broadcast(128)), w=["Gb"], dma="c2")
        A("sp", lambda e: e.dma_start(out=Bb[:], in_=ln_in_b.partition_broadcast(128)), w=["Bb"], dma="c3")

        xt = ar.alloc("xt", [128, 2, D], F32)
        xn = ar.alloc("xn", [128, 2, D], F32)
        xg = ar.alloc("xg", [128, 2, D], F32)
        hb = ar.alloc("hb", [128, 2, D], BF16)
        stt = ar.alloc("stt", [128, 2, 2, 6], F32)
        mv = ar.alloc("mv", [128, 2, 2], F32)
        sd = ar.alloc("sd", [128, 2, 1], F32)
        wst = ar.alloc("wst", [128, 2, INC], F32)
        wsq = ar.alloc("wsq", [128, 3, 768], F32)
        wskv = ar.alloc("wskv", [128, 2, 1024], F32)
        A("sp", lambda e: e.dma_start(out=wsq[:], in_=w_q_b.rearrange("(kc p) n -> p kc n", p=128)), w=["wsq"], dma="c5")
        A("sp", lambda e: e.dma_start(out=wskv[:], in_=w_kv_b.rearrange("(kc p) n -> p kc n", p=128)), w=["wskv"], dma="c6")
        A("pool", lambda e: e.memset(Wq2[:], 0.0), w=["Wq2"])
        A("pool", lambda e: e.memset(Wq[:], 0.0), w=["Wq"])
        for kc in range(3):
            wv_ = lambda kc=kc: wsq[:, kc, :].rearrange("p (h c) -> p h c", c=96)
            A("dve", lambda e, kc=kc, wv_=wv_: e.tensor_scalar(out=Wq[:, kc, :, 0:64], in0=wv_()[:, :, 0:64], scalar1=pcol[:, QG + kc:QG + kc + 1], scalar2=None, op0=ALU.mult), r=["wsq", "pcol"], w=["Wq"])
            A("dve", lambda e, kc=kc, wv_=wv_: e.tensor_scalar(out=Wq[:, kc, :, 96:128], in0=wv_()[:, :, 64:96], scalar1=pcol[:, QG + kc:QG + kc + 1], scalar2=None, op0=ALU.mult), r=["wsq", "pcol"], w=["Wq"])
            A("pool", lambda e, kc=kc: e.tensor_scalar(out=Wq2[:, kc, :, 96:112], in0=Wq[:, kc, :, 112:128], scalar1=-1.0, scalar2=None, op0=ALU.mult), r=["Wq"], w=["Wq2"])
            A("pool", lambda e, kc=kc: e.tensor_copy(out=Wq2[:, kc, :, 112:128], in_=Wq[:, kc, :, 96:112]), r=["Wq"], w=["Wq2"])
        for kc in range(2):
            A("dve", lambda e, kc=kc: e.tensor_scalar(out=Wk[:, kc, :, :], in0=wskv[:, kc, :].rearrange("p (h c) -> p h c", c=128)[:, :, 0:64], scalar1=pcol[:, KVG + kc:KVG + kc + 1], scalar2=None, op0=ALU.mult), r=["wskv", "pcol"], w=["Wk"])
            A("dve", lambda e, kc=kc: e.tensor_scalar(out=Wv[:, kc, :, :], in0=wskv[:, kc, :].rearrange("p (h c) -> p h c", c=128)[:, :, 64:128], scalar1=pcol[:, KVG + kc:KVG + kc + 1], scalar2=None, op0=ALU.mult), r=["wskv", "pcol"], w=["Wv"])

        def ln_stats(src_fn, s, key_src, rs_ap, nm_ap, key_out, eps=1e-5):
            for hh in range(2):
                A("dve", lambda e, hh=hh: e.bn_stats(out=stt[:, s, hh, :], in_=src_fn()[:, hh * 512:(hh + 1) * 512]), r=[key_src], w=[("stt", s, hh)])
            A("dve", lambda e: e.bn_aggr(out=mv[:, s, :], in_=stt[:, s, :, :]), r=[("stt", s, 0), ("stt", s, 1)], w=[("mv", s)])
            A("act", lambda e: e.activation(out=sd[:, s, :], in_=mv[:, s, 1:2], func=AF.Sqrt, bias=eps, scale=1.0), r=[("mv", s)], w=[("sd", s)])
            A("dve", lambda e: e.reciprocal(out=rs_ap, in_=sd[:, s, :]), r=[("sd", s)], w=[key_out + ("rs",)])
            A("dve", lambda e: e.scalar_tensor_tensor(out=nm_ap, in0=mv[:, s, 0:1], scalar=-1.0, in1=rs_ap, op0=ALU.mult, op1=ALU.mult), r=[("mv", s), key_out + ("rs",)], w=[key_out + ("nm",)])

        def load_win(kc):
            s = kc % 2
            A("sp", lambda e: e.dma_start(out=wst[:, s, :], in_=w_in[kc * 128:(kc + 1) * 128, :]), w=[("wst", s)], dma=f"wst{s}")
            A("act", lambda e: e.activation(out=Win[:, kc, :], in_=wst[:, s, :], func=AF.Copy), r=[("wst", s)], w=[("Win", kc)])

        for i in range(NT):
            s = i % 2
            A("sp", lambda e, i=i, s=s: e.dma_start(out=xt[:, s, :], in_=x[i * 128:(i + 1) * 128, :]), w=[("xt", s)], dma=f"x{s}")
            if i < 8:
                load_win(i)
            ln_stats(lambda s=s: xt[:, s, :], s, ("xt", s), stat_rs[:, i:i + 1], stat_nm[:, i:i + 1], ("st", i))
            A("act", lambda e, s=s, i=i: e.activation(out=xn[:, s, :], in_=xt[:, s, :], func=AF.Identity, bias=stat_nm[:, i:i + 1], scale=stat_rs[:, i:i + 1]), r=[("xt", s), ("st", i, "rs"), ("st", i, "nm")], w=[("xn", s)])
            A("dve", lambda e, s=s: e.tensor_tensor(out=xg[:, s, :], in0=xn[:, s, :], in1=Gb[:], op=ALU.mult), r=[("xn", s), "Gb"], w=[("xg", s)])
            A("dve", lambda e, s=s: e.tensor_tensor(out=hb[:, s, :], in0=xg[:, s, :], in1=Bb[:], op=ALU.add), r=[("xg", s), "Bb"], w=[("hb", s)])
            for kc in range(8):
                A("pe", lambda e, s=s, kc=kc: e.transpose(out=psb16(s)[:, kc * 128:(kc + 1) * 128], in_=hb[:, s, kc * 128:(kc + 1) * 128], identity=identb[:]), r=[("hb", s), "identb"], w=[("ps", s)])
            A("act", lambda e, s=s, i=i: e.activation(out=hT[:, :, i * 128:(i + 1) * 128], in_=psb16(s).rearrange("p (k t) -> p k t", k=8), func=AF.Copy), r=[("ps", s)], w=[("hT", i // 4)])
        tap("hT", lambda: hT[:, 0, :], [128, S], BF16, [("hT", g) for g in range(4)])
        A("dve", lambda e: e.memset(Wkr2[:], 0.0), w=["Wkr2"])
        A("dve", lambda e: e.tensor_scalar(out=Wkr2[:, :, 96:112], in0=Win[:, :, 656:672], scalar1=-1.0, scalar2=None, op0=ALU.mult), r=[("Win", k) for k in range(8)], w=["Wkr2"])
        A("dve", lambda e: e.tensor_copy(out=Wkr2[:, :, 112:128], in_=Win[:, :, 640:656]), r=[("Win", k) for k in range(8)], w=["Wkr2"])
        WinK = [("Win", k) for k in range(8)]
        sc.barrier()
        LN_PEAK = ar.cur
        if stop_after == 'ln':
            return

        ar = Arena(nc, Y_START)
        scanmask = ar.alloc("scanmask", [128, S], F32)
        A("pool", lambda e: e.memset(scanmask[:], 1.0), w=["scanmask"])
        A("pool", lambda e: e.memset(scanmask[:, 0:S:64], 0.0), w=["scanmask"])
        qsT = ar.alloc("qsT", [128, S], BF16)
        gsT = ar.alloc("gsT", [128, S], BF16)
        fT = ar.alloc("fT", [128, S], F32)
        lfT = ar.alloc("lfT", [128, S], F32)
        bT = ar.alloc("bT", [128, S], F32)
        ebT = ar.alloc("ebT", [128, S], F32)
        qbT = qsT
        qbK = [("qsT", g) for g in range(4)]
        kbT = ar.alloc("kbT", [128, S], BF16)
        vtok = ar.alloc("vtok", [128, NT, 128], BF16)
        kbtok = ar.alloc("kbtok", [128, NT, 128], BF16)
        dcol = ar.alloc("dcol", [128, 32], F32)
        sig = ar.alloc("sig", [128, 2, 512], F32)
        scm = ar.alloc("scm", [128, 2, 128], BF16)
        Tst = ar.alloc("Tst", [128, 2, 128], F32)
        stbf = ar.alloc("stbf", [128, 2, 128], BF16)
        osq = ar.alloc("osq", [128, 512], BF16)
        lnv = ar.alloc("lnv", [128, 512], F32)
        Rv = ar.alloc("Rv", [128, 512], F32)
        ot = ar.alloc("ot", [128, 512], F32)
        for h in range(4):
            cq, cf, ci, cg = 672 + h * 128, 1184 + h * 128, 1696 + h * 128, 2208 + h * 128
            n = 0
            for tg in range(4):
                tsl = slice(tg * 512, (tg + 1) * 512)
                for which, c0 in (("q", cq), ("g", cg), ("f", cf)):
                    b = n % 2
                    n += 1
                    for kc in range(8):
                        A("pe", lambda e, b=b, kc=kc, c0=c0, tsl=tsl: e.matmul(psb(b), lhsT=Win[:, kc, c0:c0 + 128], rhs=hT[:, kc, tsl], start=(kc == 0), stop=(kc == 7)), r=WinK + [("hT", tg)], w=[("ps", b)])
                    if which == "q":
                        A("act", lambda e, b=b, tsl=tsl: e.activation(out=qsT[:, tsl], in_=psb(b), func=AF.Silu), r=[("ps", b)], w=[("qsT", tg)])
                    elif which == "g":
                        A("act", lambda e, b=b, tsl=tsl: e.activation(out=gsT[:, tsl], in_=psb(b), func=AF.Silu), r=[("ps", b)], w=[("gsT", tg)])
                    else:
                        A("act", lambda e, b=b: e.activation(out=sig[:, b, :], in_=psb(b), func=AF.Sigmoid), r=[("ps", b)], w=[("sig", b)])
                        A("dve", lambda e, b=b, tsl=tsl, h=h: e.tensor_scalar(out=fT[:, tsl], in0=sig[:, b, :], scalar1=omlb[:, h:h + 1], scalar2=lbc[:, h:h + 1], op0=ALU.mult, op1=ALU.add), r=[("sig", b), "omlb", "lbc"], w=[("fT", tg)])
            fTk = [("fT", g) for g in range(4)]
            for i in range(NT):
                if i % 4 == 0:
                    pass
                for kc in range(8):
                    A("pe", lambda e, i=i, kc=kc, ci=ci: e.matmul(ps[:, 2, (i % 4) * 128:(i % 4 + 1) * 128], lhsT=hT[:, kc, i * 128:(i + 1) * 128], rhs=Win[:, kc, ci:ci + 128], start=(kc == 0), stop=(kc == 7)), r=WinK + [("hT", i // 4)], w=[("ps", 2)])
                if i % 4 == 3:
                    A("dve", lambda e, i=i: e.tensor_copy(out=vtok[:, i - 3:i + 1, :], in_=psb(2).rearrange("p (a b) -> p a b", a=4)), r=[("ps", 2)], w=["vtok"])
            A("act", lambda e: e.activation(out=lfT[:], in_=fT[:], func=AF.Ln), r=fTk, w=["lfT"])
            A("dve", lambda e: e.tensor_tensor_scan(out=bT[:], data0=scanmask[:], data1=lfT[:], initial=0.0, op0=ALU.mult, op1=ALU.add), r=["scanmask", "lfT"], w=["bT"])
            A("act", lambda e: e.activation(out=ebT[:], in_=bT[:], func=AF.Exp), r=["bT"], w=["ebT"])
            A("act", lambda e: e.activation(out=lfT[:], in_=bT[:], func=AF.Exp, scale=-1.0), r=["bT"], w=["lfT"])
            A("act", lambda e: e.activation(out=fT[:], in_=fT[:], func=AF.Identity, bias=1.0, scale=-1.0), r=fTk, w=fTk)
            A("dve", lambda e: e.tensor_tensor(out=qbT[:], in0=qsT[:], in1=ebT[:], op=ALU.mult), r=[("qsT", g) for g in range(4)] + ["ebT"], w=qbK)
            A("dve", lambda e: e.tensor_tensor(out=kbT[:], in0=fT[:], in1=lfT[:], op=ALU.mult), r=fTk + ["lfT"], w=["kbT"])
            A("dve", lambda e: e.tensor_copy(out=dcol[:], in_=ebT[:, 63:S:64]), r=["ebT"], w=["dcol"])
            for i in range(NT):
                A("pe", lambda e, i=i: e.transpose(out=psb16(2)[:, (i % 4) * 128:(i % 4 + 1) * 128], in_=kbT[:, i * 128:(i + 1) * 128], identity=identb[:]), r=["kbT", "identb"], w=[("ps", 2)])
                if i % 4 == 3:
                    A("act", lambda e, i=i: e.activation(out=kbtok[:, i - 3:i + 1, :], in_=psb16(2)[:, 0:512].rearrange("p (a b) -> p a b", a=4), func=AF.Copy), r=[("ps", 2)], w=["kbtok"])
            for i in range(NT):
                tsl = slice(i * 128, (i + 1) * 128)
                sb_ = i % 2
                A("pe", lambda e, tsl=tsl: e.matmul(ps[:, 3, 0:128], lhsT=kbT[:, tsl], rhs=qbT[:, tsl], start=True, stop=True), r=["kbT"] + qbK, w=[("ps", 3)])
                A("dve", lambda e, sb_=sb_: e.tensor_tensor(out=scm[:, sb_, :], in0=ps[:, 3, 0:128], in1=m2, op=ALU.mult), r=[("ps", 3), "cs"], w=[("scm", sb_)])
                for cc in range(2):
                    c = 2 * i + cc
                    pb_ = 4 + (c % 2)
                    lo = cc * 64
                    A("pe", lambda e, pb_=pb_, lo=lo, i=i: e.matmul(ps[:, pb_, 0:128], lhsT=kbtok[lo:lo + 64, i, :], rhs=vtok[lo:lo + 64, i, :], start=True, stop=True), r=["kbtok", "vtok"], w=[("ps", pb_)])
                oc = (i % 4) * 128
                A("pe", lambda e, i=i, sb_=sb_, oc=oc: e.matmul(ps[:, 6, oc:oc + 128], lhsT=vtok[:, i, :], rhs=scm[:, sb_, :], start=True, stop=False), r=["vtok", ("scm", sb_)], w=[("ps", 6)])
                for cc in range(2):
                    c = 2 * i + cc
                    pb_ = 4 + (c % 2)
                    tb = c % 2
                    if c > 0:
                        A("pe", lambda e, c=c, oc=oc, cc=cc: e.matmul(ps[:, 6, oc + cc * 64:oc + cc * 64 + 64], lhsT=stbf[:, (c - 1) % 2, :], rhs=qbT[:, c * 64:(c + 1) * 64], start=False, stop=(cc == 1)), r=[("stbf", (c - 1) % 2)] + qbK, w=[("ps", 6)])
                    if c == 0:
                        A("dve", lambda e, pb_=pb_, tb=tb: e.tensor_copy(out=Tst[:, tb, :], in_=ps[:, pb_, 0:128]), r=[("ps", pb_)], w=[("Tst", tb)])
                    else:
                        A("dve", lambda e, pb_=pb_, tb=tb, c=c: e.scalar_tensor_tensor(out=Tst[:, tb, :], in0=Tst[:, 1 - tb, :], scalar=dcol[:, c - 1:c], in1=ps[:, pb_, 0:128], op0=ALU.mult, op1=ALU.add), r=[("ps", pb_), ("Tst", 1 - tb), "dcol"], w=[("Tst", tb)])
                    if c < 31:
                        A("act", lambda e, tb=tb, c=c: e.activation(out=stbf[:, tb, :], in_=Tst[:, tb, :], func=AF.Identity, scale=dcol[:, c:c + 1]), r=[("Tst", tb), "dcol"], w=[("stbf", tb)])
                if i % 4 == 3:
                    tg = i // 4
                    tsl4 = slice(tg * 512, (tg + 1) * 512)
                    A("act", lambda e: e.activation(out=osq[:], in_=psb(6), func=AF.Square), r=[("ps", 6)], w=["osq"])
                    A("pe", lambda e: e.matmul(psb(7), lhsT=onesb[:], rhs=osq[:], start=True, stop=True), r=["onesb", "osq"], w=[("ps", 7)])
                    A("act", lambda e: e.activation(out=lnv[:], in_=psb(7), func=AF.Ln, bias=1e-6, scale=1.0 / 128), r=[("ps", 7)], w=["lnv"])
                    A("act", lambda e: e.activation(out=Rv[:], in_=lnv[:], func=AF.Exp, scale=-0.5), r=["lnv"], w=["Rv"])
                    A("dve", lambda e: e.tensor_tensor(out=ot[:], in0=psb(6), in1=Rv[:], op=ALU.mult), r=[("ps", 6), "Rv"], w=["ot"])
                    A("dve", lambda e, h=h, tsl4=tsl4: e.tensor_tensor(out=mixT[:, 4 + h, tsl4], in0=ot[:], in1=gsT[:, tsl4], op=ALU.mult), r=["ot", ("gsT", tg)], w=[("mixT", 4 + h, tg)])
        tap("mixh", lambda: mixT[:, 4:8, :], [128, 4, S], BF16, [("mixT", 4 + h, g) for h in range(4) for g in range(4)])
        sc.barrier()

        if stop_after == 'hgrn':
            return
        ar = Arena(nc, Y_START)
        qaT = ar.alloc("qaT", [128, 3, S], BF16)
        kvT = ar.alloc("kvT", [128, 2, S], BF16)
        cosT = ar.alloc("cosT", [128, S], F32)
        sinT = ar.alloc("sinT", [128, S], F32)
        KR = ar.alloc("KR", [128, S], BF16)
        MLA_END = ar.cur
        posi = ar.alloc("posi", [128, 512], I32)
        ang = ar.alloc("ang", [128, 512], F32)
        tk = ar.alloc("tk", [128, 512], F32)
        tki = ar.alloc("tki", [128, 512], I32)
        tr_ = ar.alloc("tr_", [128, 512], F32)
        sqq = ar.alloc("sqq", [128, 3, 512], BF16)
        qraw = ar.alloc("qraw", [128, 3, 512], F32)
        lnq = ar.alloc("lnq", [128, 512], F32)
        Rt = ar.alloc("Rt", [128, 512], F32)
        tr1 = ar.alloc("tr1", [128, 512], F32)
        tr2 = ar.alloc("tr2", [128, 512], F32)
        R = slice(96, 128)

        def sincos(dst, shift, key, tsl, tg):
            if shift != 0.0:
                A("pool", lambda e: e.tensor_scalar(out=tr_[R, :], in0=ang[R, :], scalar1=shift, scalar2=None, op0=ALU.add), r=["ang"], w=["tr_"])
            else:
                A("pool", lambda e: e.tensor_copy(out=tr_[R, :], in_=ang[R, :]), r=["ang"], w=["tr_"])
            A("dve", lambda e: e.tensor_scalar(out=tk[R, :], in0=tr_[R, :], scalar1=1.0 / TWO_PI, scalar2=None, op0=ALU.mult), r=["tr_"], w=["tk"])
            A("dve", lambda e: e.tensor_copy(out=tki[R, :], in_=tk[R, :]), r=["tk"], w=["tki"])
            A("dve", lambda e: e.tensor_copy(out=tk[R, :], in_=tki[R, :]), r=["tki"], w=["tk"])
            A("dve", lambda e: e.scalar_tensor_tensor(out=tr_[R, :], in0=tk[R, :], scalar=-C1, in1=tr_[R, :], op0=ALU.mult, op1=ALU.add), r=["tk", "tr_"], w=["tr_"])
            A("dve", lambda e: e.scalar_tensor_tensor(out=tr_[R, :], in0=tk[R, :], scalar=-C2, in1=tr_[R, :], op0=ALU.mult, op1=ALU.add), r=["tk", "tr_"], w=["tr_"])
            A("dve", lambda e: e.tensor_scalar(out=tk[R, :], in0=tr_[R, :], scalar1=PI, scalar2=-TWO_PI, op0=ALU.is_gt, op1=ALU.mult), r=["tr_"], w=["tk"])
            A("dve", lambda e: e.tensor_tensor(out=tr_[R, :], in0=tr_[R, :], in1=tk[R, :], op=ALU.add), r=["tr_", "tk"], w=["tr_"])
            A("dve", lambda e: e.tensor_scalar(out=tk[R, :], in0=tr_[R, :], scalar1=-PI, scalar2=TWO_PI, op0=ALU.is_lt, op1=ALU.mult), r=["tr_"], w=["tk"])
            A("dve", lambda e: e.tensor_tensor(out=tr_[R, :], in0=tr_[R, :], in1=tk[R, :], op=ALU.add), r=["tr_", "tk"], w=["tr_"])
            A("dve", lambda e: e.tensor_scalar(out=tr_[R, :], in0=tr_[R, :], scalar1=PI, scalar2=-PI, op0=ALU.min, op1=ALU.max), r=["tr_"], w=["tr_"])
            A("act", lambda e: e.activation(out=dst[R, tsl], in_=tr_[R, :], func=AF.Sin), r=["tr_"], w=[(key, tg)])

        for tg in range(4):
            tsl = slice(tg * 512, (tg + 1) * 512)
            A("sp", lambda e, tsl=tsl: e.dma_start(out=posi[R, :], in_=pos[:, tsl].partition_broadcast(32)), w=["posi"], dma="c4")
            A("dve", lambda e: e.tensor_copy(out=ang[R, :], in_=posi[R, :]), r=["posi"], w=["ang"])
            A("dve", lambda e: e.tensor_scalar(out=ang[R, :], in0=ang[R, :], scalar1=invf[R, :], scalar2=None, op0=ALU.mult), r=["ang", "cs"], w=["ang"])
            sincos(sinT, 0.0, "sinT", tsl, tg)
            sincos(cosT, PI / 2, "cosT", tsl, tg)
        cosK = [("cosT", g) for g in range(4)]
        sinK = [("sinT", g) for g in range(4)]
        tap("cos", lambda: cosT[96:112, :], [16, S], F32, cosK)
        tap("sin", lambda: sinT[96:112, :], [16, S], F32, sinK)
        if stop_after == 'rope':
            return

        n = 0
        for tg in range(4):
            tsl = slice(tg * 512, (tg + 1) * 512)
            for which, nj, c0, dstT, width in (("q", 3, 0, qaT, 384.0), ("kv", 2, 384, kvT, 256.0)):
                for j in range(nj):
                    b = n % 2
                    n += 1
                    for kc in range(8):
                        A("pe", lambda e, b=b, kc=kc, c=c0 + j * 128, tsl=tsl: e.matmul(psb(b), lhsT=Win[:, kc, c:c + 128], rhs=hT[:, kc, tsl], start=(kc == 0), stop=(kc == 7)), r=WinK + [("hT", tg)], w=[("ps", b)])
                    A("dve", lambda e, b=b, j=j: e.tensor_copy(out=qraw[:, j, :], in_=psb(b)), r=[("ps", b)], w=[("qraw", j)])
                    A("act", lambda e, b=b, j=j: e.activation(out=sqq[:, j, :], in_=psb(b), func=AF.Square), r=[("ps", b)], w=[("sqq", j)])
                for j in range(nj):
                    A("pe", lambda e, j=j, nj=nj: e.matmul(psb(2), lhsT=onesb[:], rhs=sqq[:, j, :], start=(j == 0), stop=(j == nj - 1)), r=["onesb", ("sqq", j)], w=[("ps", 2)])
                A("act", lambda e, width=width: e.activation(out=lnq[:], in_=psb(2), func=AF.Ln, bias=1e-6, scale=1.0 / width), r=[("ps", 2)], w=["lnq"])
                A("act", lambda e: e.activation(out=Rt[:], in_=lnq[:], func=AF.Exp, scale=-0.5), r=["lnq"], w=["Rt"])
                for j in range(nj):
                    A("dve", lambda e, j=j, dstT=dstT, tsl=tsl: e.tensor_tensor(out=dstT[:, j, tsl], in0=qraw[:, j, :], in1=Rt[:], op=ALU.mult), r=[("qraw", j), "Rt"], w=[(which + "T", tg)])
            if stop_after == 'mla_nokr':
                continue
            for kc in range(8):
                A("pe", lambda e, kc=kc, tsl=tsl: e.matmul(psb(4), lhsT=Win[:, kc, 544:672], rhs=hT[:, kc, tsl], start=(kc == 0), stop=(kc == 7)), r=WinK + [("hT", tg)], w=[("ps", 4)])
            for kc in range(8):
                A("pe", lambda e, kc=kc, tsl=tsl: e.matmul(psb(5), lhsT=Wkr2[:, kc, :], rhs=hT[:, kc, tsl], start=(kc == 0), stop=(kc == 7)), r=["Wkr2", ("hT", tg)], w=[("ps", 5)])
            A("dve", lambda e, tsl=tsl: e.tensor_tensor(out=tr1[R, :], in0=ps[R, 4, :], in1=cosT[R, tsl], op=ALU.mult), r=[("ps", 4), ("cosT", tg)], w=["tr1"])
            A("dve", lambda e, tsl=tsl: e.tensor_tensor(out=tr2[R, :], in0=ps[R, 5, :], in1=sinT[R, tsl], op=ALU.mult), r=[("ps", 5), ("sinT", tg)], w=["tr2"])
            A("pool", lambda e, tsl=tsl: e.tensor_tensor(out=KR[R, tsl], in0=tr1[R, :], in1=tr2[R, :], op=ALU.add), r=["tr1", "tr2"], w=[("KR", tg)])
        tap("qaT", lambda: qaT[:, 0, :], [128, S], BF16, [("qT", g) for g in range(4)])
        if stop_after == 'mla_nokr':
            return
        tap("KR", lambda: KR[96:128, :], [32, S], BF16, [("KR", g) for g in range(4)])
        sc.barrier()
        MLA_PEAK = ar.cur
        if stop_after == 'mla':
            return

        ar = Arena(nc, CW_END)
        QT = ar.alloc("QT", [128, 8, S], BF16)
        KT = ar.alloc("KT", [128, 8, S], BF16)
        t1 = ar.alloc("t1", [128, 2, 512], F32)
        t2 = ar.alloc("t2", [128, 2, 512], F32)
        assert ar.cur <= Y_START, (ar.cur, Y_START)
        ar = Arena(nc, MLA_END)
        Vz = ar.alloc("Vz", [128, NT, 17 * 64], BF16)
        A("dve", lambda e: e.memset(Vz[:], 0.0), w=["Vz"])
        A("dve", lambda e: e.memset(KT[64:96, :, :], 0.0), w=[("KT", h) for h in range(8)])
        for tg in range(4):
            tsl = slice(tg * 512, (tg + 1) * 512)
            for h in range(8):
                pb1, pb2, pbk = (h % 2) * 2, (h % 2) * 2 + 1, 4 + h % 2
                tb = h % 2
                for kc in range(3):
                    A("pe", lambda e, kc=kc, h=h, pb1=pb1, tsl=tsl: e.matmul(psb(pb1), lhsT=Wq[:, kc, h, :], rhs=qaT[:, kc, tsl], start=(kc == 0), stop=(kc == 2)), r=["Wq", ("qT", tg)], w=[("ps", pb1)])
                for kc in range(3):
                    A("pe", lambda e, kc=kc, h=h, pb2=pb2, tsl=tsl: e.matmul(psb(pb2), lhsT=Wq2[:, kc, h, :], rhs=qaT[:, kc, tsl], start=(kc == 0), stop=(kc == 2)), r=["Wq2", ("qT", tg)], w=[("ps", pb2)])
                for kc in range(2):
                    A("pe", lambda e, kc=kc, h=h, pbk=pbk, tsl=tsl: e.matmul(ps[0:64, pbk, :], lhsT=Wk[:, kc, h, :], rhs=kvT[:, kc, tsl], start=(kc == 0), stop=(kc == 1)), r=["Wk", ("kvT", tg)], w=[("ps", pbk)])
                A("act", lambda e, h=h, pb1=pb1, tsl=tsl: e.activation(out=QT[0:96, h, tsl], in_=ps[0:96, pb1, :], func=AF.Copy), r=[("ps", pb1)], w=[("QT", h)])
                A("dve", lambda e, pb1=pb1, tb=tb, tsl=tsl: e.tensor_tensor(out=t1[R, tb, :], in0=ps[R, pb1, :], in1=cosT[R, tsl], op=ALU.mult), r=[("ps", pb1), ("cosT", tg)], w=[("t1", tb)])
                A("dve", lambda e, pb2=pb2, tb=tb, tsl=tsl: e.tensor_tensor(out=t2[R, tb, :], in0=ps[R, pb2, :], in1=sinT[R, tsl], op=ALU.mult), r=[("ps", pb2), ("sinT", tg)], w=[("t2", tb)])
                A("dve", lambda e, h=h, tb=tb, tsl=tsl: e.tensor_tensor(out=QT[R, h, tsl], in0=t1[R, tb, :], in1=t2[R, tb, :], op=ALU.add), r=[("t1", tb), ("t2", tb)], w=[("QT", h)])
                A("act", lambda e, h=h, pbk=pbk, tsl=tsl: e.activation(out=KT[0:64, h, tsl], in_=ps[0:64, pbk, :], func=AF.Copy), r=[("ps", pbk)], w=[("KT", h)])
                A("act", lambda e, h=h, tsl=tsl: e.activation(out=KT[R, h, tsl], in_=KR[R, tsl], func=AF.Copy), r=[("KR", tg)], w=[("KT", h)])
        for i in range(NT):
            pbv = 6 + i % 2
            for kc in range(2):
                A("pe", lambda e, kc=kc, i=i, pbv=pbv: e.matmul(psb(pbv), lhsT=kvT[:, kc, i * 128:(i + 1) * 128], rhs=Wv[:, kc, :, :], start=(kc == 0), stop=(kc == 1)), r=["Wv", ("kvT", i // 4)], w=[("ps", pbv)])
            A("dve", lambda e, i=i, pbv=pbv: e.tensor_copy(out=Vz[:, i, 64:64 + 1024].rearrange("p (h c) -> p h c", c=128)[:, :, 0:64], in_=psb(pbv).rearrange("p (h c) -> p h c", c=64)), r=[("ps", pbv)], w=["Vz"])
        tap("QT0", lambda: QT[:, 0, :], [128, S], BF16, [("QT", 0)])
        tap("KT0", lambda: KT[:, 0, :], [128, S], BF16, [("KT", 0)])
        tap("V", lambda: Vz[:, 0, :], [128, 17 * 64], BF16, ["Vz"])
        sc.barrier()

        if stop_after == 'c':
            return
        ar = Arena(nc, Y_START)
        PT = [ar.alloc(f"PT{k}", [128, 512], BF16) for k in range(3)]
        Dinv = ar.alloc("Dinv", [128, 512], F32)
        attf = ar.alloc("attf", [128, 4, 512], F32)
        asq = ar.alloc("asq", [128, 2, 512], BF16)
        lnr = ar.alloc("lnr", [128, 512], F32)
        Ra = ar.alloc("Ra", [128, 512], F32)
        scale = 96.0 ** -0.5
        its = []
        it = 0
        for g in range(4):
            for pair in range(4):
                po_, pd_ = 3 + it % 2, 5 + it % 2
                it += 1
                nk = 4 * g + 4
                for kt in range(nk):
                    for hh in range(2):
                        its.append(dict(g=g, pair=pair, kt=kt, hh=hh, po=po_, pd=pd_, first=(kt == 0 and hh == 0), last=(kt == nk - 1 and hh == 1)))
        LA = 2
        deferred = []

        def emit_S(n):
            d = its[n]
            g, kt, h = d["g"], d["kt"], 2 * d["pair"] + d["hh"]
            q0 = max(g * 512, kt * 128)
            ncols = (g + 1) * 512 - q0
            sb_ = n % 3
            A("pe", lambda e: e.matmul(ps[:, sb_, 0:ncols], lhsT=KT[:, h, kt * 128:(kt + 1) * 128], rhs=QT[:, h, q0:q0 + ncols], start=True, stop=True), r=[("KT", h), ("QT", h)], w=[("ps", sb_)])
            A("act", lambda e: e.activation(out=PT[sb_][:, 0:ncols], in_=ps[:, sb_, 0:ncols], func=AF.Exp, scale=scale), r=[("ps", sb_)], w=[("PT", sb_)])
            if kt >= 4 * g:
                A("pool", lambda e: e.memset(PT[sb_][64:128, 0:64], 0.0), w=[("PT", sb_)])

        def emit_PV(n):
            d = its[n]
            g, kt, hh, pair = d["g"], d["kt"], d["hh"], d["pair"]
            h = 2 * pair + hh
            po_, pd_, first, last = d["po"], d["pd"], d["first"], d["last"]
            q0 = max(g * 512, kt * 128)
            ncols = (g + 1) * 512 - q0
            c0 = q0 - g * 512
            sb_ = n % 3
            vs = h * 128 + 64 if hh == 0 else h * 128
            os_ = 64 if hh == 0 else 0
            A("pe", lambda e: e.matmul(ps[:, po_, c0:c0 + ncols], lhsT=Vz[:, kt, vs:vs + 128], rhs=PT[sb_][:, 0:ncols], start=first, stop=last), r=["Vz", ("PT", sb_)], w=[("ps", po_)])
            A("pe", lambda e: e.matmul(ps[:, pd_, c0:c0 + ncols], lhsT=oz[:, os_:os_ + 128], rhs=PT[sb_][:, 0:ncols], start=first, stop=last), r=["oz", ("PT", sb_)], w=[("ps", pd_)])
            if last:
                A("dve", lambda e: e.reciprocal(out=Dinv[:], in_=psb(pd_)), r=[("ps", pd_)], w=["Dinv"])
                A("dve", lambda e: e.tensor_tensor(out=attf[:, pair, :], in0=psb(po_), in1=Dinv[:], op=ALU.mult), r=[("ps", po_), "Dinv"], w=[("attf", pair)])
                A("act", lambda e: e.activation(out=asq[:, pair % 2, :], in_=attf[:, pair, :], func=AF.Square), r=[("attf", pair)], w=[("asq", pair % 2)])

                def fin():
                    A("pe", lambda e: e.matmul(psb(7), lhsT=onesb[:], rhs=asq[:, pair % 2, :], start=(pair == 0), stop=(pair == 3)), r=["onesb", ("asq", pair % 2)], w=[("ps", 7)])
                    if pair == 3:
                        A("act", lambda e: e.activation(out=lnr[:], in_=psb(7), func=AF.Ln, bias=1e-6, scale=1.0 / 512), r=[("ps", 7)], w=["lnr"])
                        A("act", lambda e: e.activation(out=Ra[:], in_=lnr[:], func=AF.Exp, scale=-0.5), r=["lnr"], w=["Ra"])
                        for pp in range(4):
                            A("dve", lambda e, pp=pp: e.tensor_tensor(out=mixT[:, pp, g * 512:(g + 1) * 512], in0=attf[:, pp, :], in1=Ra[:], op=ALU.mult), r=[("attf", pp), "Ra"], w=[("mixT", pp, g)])
                deferred.append((n + 8, fin))

        for n in range(len(its) + LA):
            if n < len(its):
                emit_S(n)
            if n - LA >= 0:
                emit_PV(n - LA)
            while deferred and deferred[0][0] <= n - LA:
                deferred.pop(0)[1]()
        while deferred:
            deferred.pop(0)[1]()
        tap("mixa", lambda: mixT[:, 0:4, :], [128, 4, S], BF16, [("mixT", p_, g) for p_ in range(4) for g in range(4)])
        sc.barrier()

        if stop_after == 'attn':
            return
        ar = Arena(nc, P0_END)
        gate = ar.alloc("gate", [128, NT, NE], F32)
        gT = ar.alloc("gT", [32, S], F32)
        bucol = ar.alloc("bucol", [128, NE * 16], F32)
        Bd = ar.alloc("Bd", [32, D], F32)
        F_P_END = ar.cur
        Wo = ar.alloc("Wo", [128, 8, D], BF16)
        wos = ar.alloc("wos", [128, 2, D], F32)
        GbF = ar.alloc("Gb0", [128, D], F32); BbF = ar.alloc("Bb0", [128, D], F32)
        G1 = ar.alloc("G1", [128, D], F32); B1 = ar.alloc("B1", [128, D], F32)
        Wr = ar.alloc("Wr", [128, 8, NE], F32)
        brB = ar.alloc("brB", [128, NE], F32)
        xtF = ar.alloc("xt2", [128, 2, D], F32)
        xnF = ar.alloc("xn2", [128, 2, D], F32)
        hf_ = ar.alloc("hf_", [128, 2, D], F32)
        y_ = ar.alloc("y_", [128, 2, D], F32)
        h1 = ar.alloc("h1", [128, 2, D], F32)
        sttF = ar.alloc("stt2", [128, 2, 2, 6], F32)
        mvF = ar.alloc("mv2", [128, 2, 2], F32)
        sdF = ar.alloc("sd2", [128, 2, 1], F32)
        rs1 = ar.alloc("rs1", [128, 2, 1], F32)
        nm1 = ar.alloc("nm1", [128, 2, 1], F32)
        h1Tf = ar.alloc("h1Tf", [128, 8, 128], F32)
        lg = ar.alloc("lg", [128, 2, NE], F32)
        mx8 = ar.alloc("mx8", [128, 8], F32)
        nmx = ar.alloc("nmx", [128, 1], F32)
        ex = ar.alloc("ex", [128, NE], F32)
        msk = ar.alloc("msk", [128, NE], F32)
        ssum = ar.alloc("ssum", [128, 1], F32)
        bust = ar.alloc("bust", [128, 4, 128], F32)
        A("sp", lambda e: e.dma_start(out=GbF[:], in_=ln_in_g.partition_broadcast(128)), w=["Gb"], dma="c2")
        A("sp", lambda e: e.dma_start(out=BbF[:], in_=ln_in_b.partition_broadcast(128)), w=["Bb"], dma="c3")
        A("sp", lambda e: e.dma_start(out=G1[:], in_=ln1_g.partition_broadcast(128)), w=["G1"], dma="c4")
        A("sp", lambda e: e.dma_start(out=B1[:], in_=ln1_b.partition_broadcast(128)), w=["B1"], dma="c5")
        A("sp", lambda e: e.dma_start(out=Wr[:], in_=w_router.rearrange("(kc p) n -> p kc n", p=128)), w=["Wr"], dma="c6")
        A("sp", lambda e: e.dma_start(out=brB[:], in_=b_router.partition_broadcast(128)), w=["brB"], dma="c0")
        A("sp", lambda e: e.dma_start(out=Bd[:], in_=b_down), w=["Bd"], dma="c1")
        A("sp", lambda e: e.dma_start(out=bust[:], in_=b_up.rearrange("(a p) n -> p a n", p=128)), w=["bust"], dma="c7")
        for a in range(4):
            A("pe", lambda e, a=a: e.transpose(out=ps[:, 0, a * 128:(a + 1) * 128], in_=bust[:, a, :], identity=identf), r=["bust", "cs"], w=[("ps", 0)])
        A("dve", lambda e: e.tensor_copy(out=bucol[:], in_=psb(0)), r=[("ps", 0)], w=["bucol"])
        for kc in range(8):
            s = kc % 2
            A("sp", lambda e, kc=kc, s=s: e.dma_start(out=wos[:, s, :], in_=w_o[kc * 128:(kc + 1) * 128, :]), w=[("wos", s)], dma=f"wst{s}")
            gc = (MOG + kc) if kc < 4 else (HOG + kc - 4)
            A("dve", lambda e, kc=kc, s=s, gc=gc: e.tensor_scalar(out=Wo[:, kc, :], in0=wos[:, s, :], scalar1=pcol[:, gc:gc + 1], scalar2=None, op0=ALU.mult), r=[("wos", s), "pcol"], w=["Wo"])
        mixK = [("mixT", c, g) for c in range(8) for g in range(4)]
        def stA(i):
            s = i % 2
            A("sp", lambda e, i=i, s=s: e.dma_start(out=xtF[:, s, :], in_=x[i * 128:(i + 1) * 128, :]), w=[("xt", s)], dma=f"x{s}")
            A("act", lambda e, s=s, i=i: e.activation(out=xnF[:, s, :], in_=xtF[:, s, :], func=AF.Identity, bias=stat_nm[:, i:i + 1], scale=stat_rs[:, i:i + 1]), r=[("xt", s), ("st", i, "rs"), ("st", i, "nm")], w=[("xn", s)])
            A("dve", lambda e, s=s: e.tensor_tensor(out=xnF[:, s, :], in0=xnF[:, s, :], in1=GbF[:], op=ALU.mult), r=[("xn", s), "Gb"], w=[("xn", s)])
            A("dve", lambda e, s=s: e.tensor_tensor(out=hf_[:, s, :], in0=xnF[:, s, :], in1=BbF[:], op=ALU.add), r=[("xn", s), "Bb"], w=[("hf_", s)])
            for c in range(2):
                pb_ = 2 * s + c
                for kc in range(8):
                    A("pe", lambda e, i=i, kc=kc, c=c, pb_=pb_: e.matmul(psb(pb_), lhsT=mixT[:, kc, i * 128:(i + 1) * 128], rhs=Wo[:, kc, c * 512:(c + 1) * 512], start=(kc == 0), stop=(kc == 7)), r=mixK + ["Wo"], w=[("ps", pb_)])
                A("dve", lambda e, s=s, c=c, pb_=pb_: e.scalar_tensor_tensor(out=y_[:, s, c * 512:(c + 1) * 512], in0=hf_[:, s, c * 512:(c + 1) * 512], scalar=ALPHA, in1=psb(pb_), op0=ALU.mult, op1=ALU.add), r=[("hf_", s), ("ps", pb_)], w=[("y_", s, c)])
            for hh in range(2):
                A("dve", lambda e, s=s, hh=hh: e.bn_stats(out=sttF[:, s, hh, :], in_=y_[:, s, hh * 512:(hh + 1) * 512]), r=[("y_", s, hh)], w=[("stt", s, hh)])
            A("dve", lambda e, s=s: e.bn_aggr(out=mvF[:, s, :], in_=sttF[:, s, :, :]), r=[("stt", s, 0), ("stt", s, 1)], w=[("mv", s)])
            A("act", lambda e, s=s: e.activation(out=sdF[:, s, :], in_=mvF[:, s, 1:2], func=AF.Sqrt, bias=1e-5, scale=1.0), r=[("mv", s)], w=[("sd", s)])
            A("dve", lambda e, s=s: e.reciprocal(out=rs1[:, s, :], in_=sdF[:, s, :]), r=[("sd", s)], w=[("rs1", s)])
            A("dve", lambda e, s=s: e.scalar_tensor_tensor(out=nm1[:, s, :], in0=mvF[:, s, 0:1], scalar=-1.0, in1=rs1[:, s, :], op0=ALU.mult, op1=ALU.mult), r=[("mv", s), ("rs1", s)], w=[("nm1", s)])
            A("act", lambda e, s=s: e.activation(out=y_[:, s, :], in_=y_[:, s, :], func=AF.Identity, bias=nm1[:, s, :], scale=rs1[:, s, :]), r=[("y_", s, 0), ("y_", s, 1), ("rs1", s), ("nm1", s)], w=[("y_", s, 0), ("y_", s, 1)])
            A("dve", lambda e, s=s: e.tensor_tensor(out=y_[:, s, :], in0=y_[:, s, :], in1=G1[:], op=ALU.mult), r=[("y_", s, 0), ("y_", s, 1), "G1"], w=[("y_", s, 0), ("y_", s, 1)])
            A("dve", lambda e, s=s: e.tensor_tensor(out=h1[:, s, :], in0=y_[:, s, :], in1=B1[:], op=ALU.add), r=[("y_", s, 0), ("y_", s, 1), "B1"], w=[("h1", s)])
            A("sp", lambda e, i=i, s=s: e.dma_start(out=h1d[i * 128:(i + 1) * 128, :], in_=h1[:, s, :]), r=[("h1", s)], w=[("h1d", i)], dma=f"h1o{s}")

        def stB1(i):
            s = i % 2
            for kc in range(8):
                A("pe", lambda e, s=s, kc=kc: e.transpose(out=ps[:, 4 + kc // 4, (kc % 4) * 128:(kc % 4 + 1) * 128], in_=h1[:, s, kc * 128:(kc + 1) * 128], identity=identf), r=[("h1", s), "cs"], w=[("ps", 4 + kc // 4)])
            for c in range(2):
                A("act", lambda e, c=c: e.activation(out=h1Tf[:, c * 4:(c + 1) * 4, :], in_=psb(4 + c).rearrange("p (k t) -> p k t", k=4), func=AF.Copy), r=[("ps", 4 + c)], w=[("h1Tf", c)])
            for kc in range(8):
                A("pe", lambda e, kc=kc: e.matmul(ps[:, 6, 0:NE], lhsT=h1Tf[:, kc, :], rhs=Wr[:, kc, :], start=(kc == 0), stop=(kc == 7)), r=[("h1Tf", kc // 4), "Wr"], w=[("ps", 6)])
            A("dve", lambda e, s=s: e.tensor_tensor(out=lg[:, s, :], in0=ps[:, 6, 0:NE], in1=brB[:], op=ALU.add), r=[("ps", 6), "brB"], w=[("lg", s)])

        def stB2(i):
            s = i % 2
            A("dve", lambda e, s=s: e.max(out=mx8[:], in_=lg[:, s, :]), r=[("lg", s)], w=["mx8"])
            A("dve", lambda e: e.tensor_scalar(out=nmx[:], in0=mx8[:, 0:1], scalar1=-1.0, scalar2=None, op0=ALU.mult), r=["mx8"], w=["nmx"])
            A("act", lambda e, s=s: e.activation(out=ex[:], in_=lg[:, s, :], func=AF.Exp, bias=nmx[:], scale=1.0), r=[("lg", s), "nmx"], w=["ex"])
            A("dve", lambda e, s=s: e.tensor_scalar(out=msk[:], in0=lg[:, s, :], scalar1=mx8[:, 3:4], scalar2=None, op0=ALU.is_ge), r=[("lg", s), "mx8"], w=["msk"])
            A("dve", lambda e: e.tensor_tensor(out=ex[:], in0=ex[:], in1=msk[:], op=ALU.mult), r=["ex", "msk"], w=["ex"])
            A("dve", lambda e: e.reduce_sum(out=ssum[:], in_=ex[:], axis=mybir.AxisListType.X), r=["ex"], w=["ssum"])
            A("dve", lambda e: e.reciprocal(out=ssum[:], in_=ssum[:]), r=["ssum"], w=["ssum"])
            A("dve", lambda e, i=i: e.tensor_scalar(out=gate[:, i, :], in0=ex[:], scalar1=ssum[:], scalar2=None, op0=ALU.mult), r=["ex", "ssum"], w=[("gate", i)])
            A("pe", lambda e, i=i: e.transpose(out=ps[0:32, 7, 0:128], in_=gate[:, i, :], identity=identf), r=[("gate", i), "cs"], w=[("ps", 7)])
            A("act", lambda e, i=i: e.activation(out=gT[:, i * 128:(i + 1) * 128], in_=ps[0:32, 7, 0:128], func=AF.Copy), r=[("ps", 7)], w=[("gT", i)])

        for t in range(NT + 2):
            if t < NT:
                stA(t)
            if 0 <= t - 1 < NT:
                stB1(t - 1)
            if t - 2 >= 0:
                stB2(t - 2)

        tap("gate", lambda: gate[:], [128, NT, NE], F32, [("gate", i) for i in range(NT)])
        sc.barrier()
        ar = Arena(nc, F_P_END)

        if stop_after == 'f':
            return
        CAP = 384
        NB = CAP // 128
        Yacc = Arena(nc, MIX_AT).alloc("Yacc", [128, 8, D], F32)
        SCR0 = ar.cur
        actT = ar.alloc("actT", [128, 8, CAP], BF16)
        XgT = ar.alloc("XgT", [128, 8, CAP], BF16)
        Sel = ar.alloc("Sel", [128, 8, CAP], BF16)
        yo = Arena(nc, SCR0).alloc("yo", [128, 2, D], F32)
        SelT = ar.alloc("SelT", [128, NB, 1024], BF16)
        Yslot = ar.alloc("Yslot", [128, NB, D], BF16)
        h1b = ar.alloc("h1b", [128, 8, D], BF16)
        maskb = ar.alloc("maskb", [128, NT, NE], BF16)
        rankm = ar.alloc("rankm", [128, 8, NE], F32)
        strib = ar.alloc("strib", [128, 128], BF16)
        iotaC = cs[:, 400:400 + CAP]
        Wub = [ar.alloc(f"Wub{k}", [128, 8, 2048], BF16) for k in range(2)]
        Wdb = ar.alloc("Wdb", [128, 8, D], BF16)
        stg = ar.alloc("stg", [128, 2, D], F32)
        G2 = stg[:, 0, :]
        B2 = stg[:, 1, :]
        h1r = ar.alloc("h1r", [128, 1, D], F32)
        bucol1 = ar.alloc("bucol1", [128, NE * 16], F32)
        gate_s = ar.alloc("gate_s", [128, NT, NE], F32)
        gateK = [("gate", i) for i in range(NT)]
        A("dve", lambda e: e.tensor_scalar(out=bucol1[:], in0=bucol[:], scalar1=1.0, scalar2=None, op0=ALU.add), r=["bucol"], w=["bucol1"])
        A("dve", lambda e: e.tensor_scalar(out=gate_s[:], in0=gate[:], scalar1=1.0 / 1.702, scalar2=None, op0=ALU.mult), r=gateK, w=["gate_s"])
        A("dve", lambda e: e.tensor_scalar(out=maskb[:], in0=gate[:], scalar1=0.0, scalar2=None, op0=ALU.is_gt), r=gateK, w=["maskb"])
        A("dve", lambda e: e.tensor_copy(out=strib[:], in_=cs[:, 272:400]), r=["cs"], w=["strib"])
        sc.barrier()
        gt_ = ar.alloc("gt_", [128, 2, CAP], F32)
        sg_ = ar.alloc("sg_", [128, 2, CAP], F32)
        lt_ = ar.alloc("lt_", [128, 2, CAP], F32)
        sttM = ar.alloc("stt3", [128, 2, 2, 6], F32)
        mvM = ar.alloc("mv3", [128, 2, 2], F32)
        sdM = ar.alloc("sd3", [128, 2, 1], F32)
        rs2 = ar.alloc("rs2", [128, 2, 1], F32)
        nm2 = ar.alloc("nm2", [128, 2, 1], F32)
        nst = [0]

        def cast_copy(eng, out_fn, in_fn, r, w):
            if eng == "act":
                A("act", lambda e: e.activation(out=out_fn(), in_=in_fn(), func=AF.Copy), r=r, w=w)
            else:
                A(eng, lambda e: e.tensor_copy(out=out_fn(), in_=in_fn()), r=r, w=w)

        def load_up_piece(e_, wb, q):
            k = nst[0] % 2
            nst[0] += 1
            kc, ch = q // 2, q % 2
            A("sp", lambda e: e.dma_start(out=stg[:, k, :], in_=w_up[e_, kc * 128:(kc + 1) * 128, ch * 1024:(ch + 1) * 1024]), w=[("stg", k)], dma=f"stg{k}")
            cast_copy("dve" if nst[0] % 4 == 0 else "act", lambda: Wub[wb][:, kc, ch * 1024:(ch + 1) * 1024], lambda: stg[:, k, :], [("stg", k)], [("Wub", wb, kc)])

        def load_down_piece(e_, j):
            k = nst[0] % 2
            nst[0] += 1
            A("sp", lambda e: e.dma_start(out=stg[:, k, :], in_=w_down[e_, j * 128:(j + 1) * 128, :]), w=[("stg", k)], dma=f"stg{k}")
            cast_copy("dve" if nst[0] % 4 == 0 else "act", lambda: Wdb[:, j, :], lambda: stg[:, k, :], [("stg", k)], [("Wdb", j)])

        WdbK = [("Wdb", k) for k in range(8)]
        steps = [(half, e_) for half in range(2) for e_ in range(n_experts)]
        for q in range(16):
            load_up_piece(0, 0, q)
        for half in range(2):
            for il in range(8):
                i = half * 8 + il
                A("sp", lambda e, i=i: e.dma_start(out=h1r[:, 0, :], in_=h1d[i * 128:(i + 1) * 128, :]), r=[("h1d", i)], w=[("h1r", 0)], dma="x0")
                A("act", lambda e, il=il: e.activation(out=h1b[:, il, :], in_=h1r[:, 0, :], func=AF.Copy), r=[("h1r", 0)], w=[("h1b", il)])
                for c in range(2):
                    pb_ = 4 + 2 * (il % 2) + c
                    A("pe", lambda e, i=i, c=c, pb_=pb_: e.matmul(psb(pb_), lhsT=gT[:, i * 128:(i + 1) * 128], rhs=Bd[:, c * 512:(c + 1) * 512], start=True, stop=True), r=[("gT", i), "Bd"], w=[("ps", pb_)])
                    A("dve", lambda e, il=il, c=c, pb_=pb_: e.scalar_tensor_tensor(out=Yacc[:, il, c * 512:(c + 1) * 512], in0=h1r[:, 0, c * 512:(c + 1) * 512], scalar=ALPHA, in1=psb(pb_), op0=ALU.mult, op1=ALU.add), r=[("h1r", 0), ("ps", pb_)], w=[("Yacc", il, c)])
            for il in range(8):
                for jl in range(il):
                    A("pe", lambda e, il=il, jl=jl, half=half: e.matmul(ps[:, 0, il * NE:(il + 1) * NE], lhsT=onesb[:], rhs=maskb[:, half * 8 + jl, :], start=(jl == 0), stop=False), r=["onesb", "maskb"], w=[("ps", 0)])
                A("pe", lambda e, il=il, half=half: e.matmul(ps[:, 0, il * NE:(il + 1) * NE], lhsT=strib[:], rhs=maskb[:, half * 8 + il, :], start=(il == 0), stop=True), r=["strib", "maskb"], w=[("ps", 0)])
            A("dve", lambda e, half=half: e.scalar_tensor_tensor(out=rankm[:], in0=maskb[:, half * 8:half * 8 + 8, :], scalar=-4096.0, in1=ps[:, 0, 0:8 * NE].rearrange("p (a b) -> p a b", a=8), op0=ALU.mult, op1=ALU.add), r=["maskb", ("ps", 0)], w=["rankm"])
            A("dve", lambda e: e.tensor_scalar(out=rankm[:], in0=rankm[:], scalar1=4096.0, scalar2=None, op0=ALU.add), r=["rankm"], w=["rankm"])
            def emit_sel(ee):
                for il in range(8):
                    A("dve", lambda e, il=il, ee=ee: e.tensor_scalar(out=Sel[:, il, :], in0=iotaC, scalar1=rankm[:, il, ee:ee + 1], scalar2=None, op0=ALU.is_equal), r=["rankm", "cs"], w=[("Sel", il)])

            for e_ in range(n_experts):
                st_i = half * n_experts + e_
                wb = st_i % 2
                nxt = steps[st_i + 1][1] if st_i + 1 < len(steps) else None
                WubK = [("Wub", wb, k) for k in range(8)]
                pend = [(nxt, 1 - wb, q) for q in range(16)] if nxt is not None else []

                def prefetch(n):
                    for _ in range(n):
                        if pend:
                            load_up_piece(*pend.pop(0))
                if e_ == 0:
                    emit_sel(0)
                for kc in range(8):
                    bk = kc % 2
                    order = [7, 6, 5, 4, 3, 2, 1, 0]
                    for oi, il in enumerate(order):
                        nsl = min(CAP, 128 * (il + 1))
                        A("pe", lambda e, bk=bk, kc=kc, il=il, nsl=nsl, oi=oi: e.matmul(ps[:, bk, 0:nsl], lhsT=h1b[:, il, kc * 128:(kc + 1) * 128], rhs=Sel[:, il, 0:nsl], start=(oi == 0), stop=(oi == 7)), r=[("h1b", il), ("Sel", il)], w=[("ps", bk)])
                    A("act", lambda e, bk=bk, kc=kc: e.activation(out=XgT[:, kc, :], in_=ps[:, bk, 0:CAP], func=AF.Copy), r=[("ps", bk)], w=[("XgT", kc)])
                    load_down_piece(e_, kc)
                XgK = [("XgT", k) for k in range(8)]
                for sb in range(NB):
                    for il in range(8):
                        A("pe", lambda e, sb=sb, il=il: e.transpose(out=psb16(4 + sb)[:, il * 128:(il + 1) * 128], in_=Sel[:, il, sb * 128:(sb + 1) * 128], identity=identb[:]), r=[("Sel", il), "identb"], w=[("ps", 4 + sb)])
                    A("act", lambda e, sb=sb: e.activation(out=SelT[:, sb, :], in_=psb16(4 + sb), func=AF.Copy), r=[("ps", 4 + sb)], w=[("SelT", sb)])
                for j in range(8):
                    s = j % 2
                    bg_, bl_ = (2, 3) if s == 0 else (0, 1)
                    for kc in range(8):
                        A("pe", lambda e, kc=kc, j=j, bg_=bg_, wb=wb: e.matmul(ps[:, bg_, 0:CAP], lhsT=Wub[wb][:, kc, j * 128:(j + 1) * 128], rhs=XgT[:, kc, :], start=(kc == 0), stop=(kc == 7)), r=WubK + XgK, w=[("ps", bg_)])
                    for kc in range(8):
                        A("pe", lambda e, kc=kc, j=j, bl_=bl_, wb=wb: e.matmul(ps[:, bl_, 0:CAP], lhsT=Wub[wb][:, kc, 1024 + j * 128:1024 + (j + 1) * 128], rhs=XgT[:, kc, :], start=(kc == 0), stop=(kc == 7)), r=WubK + XgK, w=[("ps", bl_)])
                    bg = e_ * 16 + j
                    bl = e_ * 16 + 8 + j
                    A("dve", lambda e, s=s, bg_=bg_, bg=bg: e.tensor_scalar(out=gt_[:, s, :], in0=ps[:, bg_, 0:CAP], scalar1=bucol[:, bg:bg + 1], scalar2=7.0, op0=ALU.add, op1=ALU.min), r=[("ps", bg_), "bucol"], w=[("gt_", s)])
                    A("act", lambda e, s=s: e.activation(out=sg_[:, s, :], in_=gt_[:, s, :], func=AF.Silu, scale=1.702), r=[("gt_", s)], w=[("sg_", s)])
                    A("dve", lambda e, s=s, bl_=bl_, bl=bl: e.tensor_scalar(out=lt_[:, s, :], in0=ps[:, bl_, 0:CAP], scalar1=bucol1[:, bl:bl + 1], scalar2=-6.0, op0=ALU.add, op1=ALU.max), r=[("ps", bl_), "bucol1"], w=[("lt_", s)])
                    A("dve", lambda e, s=s, j=j: e.scalar_tensor_tensor(out=actT[:, j, :], in0=lt_[:, s, :], scalar=8.0, in1=sg_[:, s, :], op0=ALU.min, op1=ALU.mult), r=[("sg_", s), ("lt_", s)], w=[("actT", j)])
                    prefetch(1)
                actK = [("actT", j) for j in range(8)]
                for sb in range(NB):
                    for c in range(2):
                        bk = 4 + (2 * sb + c) % 2
                        for j in range(8):
                            A("pe", lambda e, sb=sb, c=c, j=j, bk=bk: e.matmul(psb(bk), lhsT=actT[:, j, sb * 128:(sb + 1) * 128], rhs=Wdb[:, j, c * 512:(c + 1) * 512], start=(j == 0), stop=(j == 7)), r=actK + WdbK, w=[("ps", bk)])
                        A("act", lambda e, sb=sb, c=c, bk=bk: e.activation(out=Yslot[:, sb, c * 512:(c + 1) * 512], in_=psb(bk), func=AF.Copy), r=[("ps", bk)], w=[("Yslot", sb, c)])
                    prefetch(1)
                if e_ + 1 < n_experts:
                    emit_sel(e_ + 1)
                for il in range(8):
                    for c in range(2):
                        bk = 6 + (2 * il + c) % 2
                        nbl = min(NB, il + 1)
                        for sb in range(nbl):
                            A("pe", lambda e, il=il, c=c, sb=sb, bk=bk, nbl=nbl: e.matmul(psb(bk), lhsT=SelT[:, sb, il * 128:(il + 1) * 128], rhs=Yslot[:, sb, c * 512:(c + 1) * 512], start=(sb == 0), stop=(sb == nbl - 1)), r=[("SelT", sb), ("Yslot", sb, c)], w=[("ps", bk)])
                        A("dve", lambda e, il=il, c=c, bk=bk, half=half, e_=e_: e.scalar_tensor_tensor(out=Yacc[:, il, c * 512:(c + 1) * 512], in0=psb(bk), scalar=gate_s[:, half * 8 + il, e_:e_ + 1], in1=Yacc[:, il, c * 512:(c + 1) * 512], op0=ALU.mult, op1=ALU.add), r=[("ps", bk), "gate_s", ("Yacc", il, c)], w=[("Yacc", il, c)])
                    if il >= 3:
                        prefetch(1)
                prefetch(16)
            sc.barrier()
            A("sp", lambda e: e.dma_start(out=G2, in_=ln2_g.partition_broadcast(128)), w=["G2"], dma="c2")
            A("sp", lambda e: e.dma_start(out=B2, in_=ln2_b.partition_broadcast(128)), w=["B2"], dma="c3")
            for il in range(8):
                i = half * 8 + il
                s = il % 2
                for hh in range(2):
                    A("dve", lambda e, s=s, hh=hh, il=il: e.bn_stats(out=sttM[:, s, hh, :], in_=Yacc[:, il, hh * 512:(hh + 1) * 512]), r=[("Yacc", il, hh)], w=[("stt", s, hh)])
                A("dve", lambda e, s=s: e.bn_aggr(out=mvM[:, s, :], in_=sttM[:, s, :, :]), r=[("stt", s, 0), ("stt", s, 1)], w=[("mv", s)])
                A("act", lambda e, s=s: e.activation(out=sdM[:, s, :], in_=mvM[:, s, 1:2], func=AF.Sqrt, bias=1e-5, scale=1.0), r=[("mv", s)], w=[("sd", s)])
                A("dve", lambda e, s=s: e.reciprocal(out=rs2[:, s, :], in_=sdM[:, s, :]), r=[("sd", s)], w=[("rs2", s)])
                A("dve", lambda e, s=s: e.scalar_tensor_tensor(out=nm2[:, s, :], in0=mvM[:, s, 0:1], scalar=-1.0, in1=rs2[:, s, :], op0=ALU.mult, op1=ALU.mult), r=[("mv", s), ("rs2", s)], w=[("nm2", s)])
                A("act", lambda e, s=s, il=il: e.activation(out=yo[:, s, :], in_=Yacc[:, il, :], func=AF.Identity, bias=nm2[:, s, :], scale=rs2[:, s, :]), r=[("Yacc", il, 0), ("Yacc", il, 1), ("rs2", s), ("nm2", s)], w=[("yo", s)])
                A("dve", lambda e, s=s: e.tensor_tensor(out=yo[:, s, :], in0=yo[:, s, :], in1=G2, op=ALU.mult), r=[("yo", s), "G2"], w=[("yo", s)])
                A("dve", lambda e, s=s: e.tensor_tensor(out=yo[:, s, :], in0=yo[:, s, :], in1=B2, op=ALU.add), r=[("yo", s), "B2"], w=[("yo", s)])
                A("sp", lambda e, i=i, s=s: e.dma_start(out=out[i * 128:(i + 1) * 128, :], in_=yo[:, s, :]), r=[("yo", s)], w=[("out", i)], dma=f"o{s}")
            sc.barrier()

    body()
    A("sp", None, r=[("out", i) for i in range(NT)] + [("tapout", k) for k in taps])

    sems = {k: nc.alloc_semaphore("s_" + k) for k in Sched.ENGS}
    dsem = {k: nc.alloc_semaphore("d_" + k) for k in dma_names}
    with nc.Block() as block:
        sc.emit(dict(pe=block.tensor, act=block.scalar, dve=block.vector, pool=block.gpsimd, sp=block.sync), sems, dsem)
    return nc, taps, 0, len(sc.ops)


def core_inputs(inputs, b):
    f = np.float32
    g = lambda k: np.ascontiguousarray(inputs[k], dtype=f)
    return {
        "x": np.ascontiguousarray(inputs["x"][b], dtype=f),
        "pos": np.ascontiguousarray(inputs["positions"][b].reshape(1, S).astype(np.int32)),
        "cst": host_consts(),
        "ln_in_g": g("ln_in_g").reshape(1, D), "ln_in_b": g("ln_in_b").reshape(1, D),
        "w_in": g("w_in").reshape(D, INC),
        "q_a_norm_g": g("q_a_norm_g").reshape(3, 128),
        "w_q_b": g("w_q_b").reshape(384, 768),
        "kv_a_norm_g": g("kv_a_norm_g").reshape(2, 128),
        "w_kv_b": g("w_kv_b").reshape(256, 1024),
        "hgrn_lb_logits": g("hgrn_lb_logits").reshape(8, 128),
        "mla_out_g": g("mla_out_g").reshape(4, 128),
        "hgrn_out_g": g("hgrn_out_g").reshape(4, 128),
        "w_o": g("w_o").reshape(D, D),
        "ln1_g": g("ln1_g").reshape(1, D), "ln1_b": g("ln1_b").reshape(1, D),
        "w_router": g("w_router").reshape(D, NE),
        "b_router": g("b_router").reshape(1, NE),
        "w_up": g("w_up").reshape(NE, D, 2048),
        "b_up": g("b_up").reshape(NE * 16, 128),
        "w_down": g("w_down").reshape(NE, D, D),
        "b_down": g("b_down").reshape(NE, D),
        "ln2_g": g("ln2_g").reshape(1, D), "ln2_b": g("ln2_b").reshape(1, D),
    }


def kernel(**inputs):
    nc, _, _, _ = build_program()
    shared = core_inputs(inputs, 0)
    in_maps = []
    for b in range(8):
        m = dict(shared)
        m["x"] = np.ascontiguousarray(inputs["x"][b], dtype=np.float32)
        m["pos"] = np.ascontiguousarray(np.asarray(inputs["positions"][b]).reshape(1, S).astype(np.int32))
        in_maps.append(m)
    res = run_bass_kernel_spmd(nc, in_maps, core_ids=list(range(8)))
    return np.stack([np.asarray(r["out"], dtype=np.float32) for r in res.results], axis=0)
```

```python
import numpy as np
import concourse.bass as bass
import concourse.mybir as mybir
from concourse.bass_utils import run_bass_kernel_spmd

F32 = mybir.dt.float32
BF16 = mybir.dt.bfloat16
I32 = mybir.dt.int32
AF = mybir.ActivationFunctionType
ALU = mybir.AluOpType

S = 2048
D = 1024
NT = 16
INC = 2720
NE = 32
ALPHA = 2.0 ** 0.25
TWO_PI = 6.283185307179586
C1 = 6.28125
C2 = TWO_PI - C1
PI = 3.141592653589793


class Sched:
    ENGS = ("pe", "act", "dve", "pool", "sp")

    def __init__(self):
        self.ops = []
        self.last_writer = {}
        self.readers = {}
        self.dma_count = {}
        self.last_dma_on_sem = {}
        self.pending_barrier = {}

    def add(self, eng, fn, reads=(), writes=(), dma_sem=None):
        i = len(self.ops)
        deps = set()
        writes = list(writes) + [k for k in reads if isinstance(k, tuple) and k and k[0] == "ps" and k not in writes]
        for k in reads:
            w = self.last_writer.get(k)
            if w is not None:
                deps.add(w)
        for k in writes:
            w = self.last_writer.get(k)
            if w is not None:
                deps.add(w)
            deps.update(self.readers.get(k, ()))
        for k in reads:
            self.readers.setdefault(k, []).append(i)
        for k in writes:
            self.last_writer[k] = i
            self.readers[k] = []
        if eng in self.pending_barrier:
            deps.update(self.pending_barrier.pop(eng))
        op = dict(eng=eng, fn=fn, deps=deps, dma_sem=dma_sem, dma_val=None)
        if dma_sem is not None:
            prev = self.last_dma_on_sem.get(dma_sem)
            if prev is not None:
                deps.add(prev)
            self.last_dma_on_sem[dma_sem] = i
            c = self.dma_count.get(dma_sem, 0) + 1
            self.dma_count[dma_sem] = c
            op["dma_val"] = 16 * c
        self.ops.append(op)
        return i

    def barrier(self):
        last = {}
        dmas = []
        for i, op in enumerate(self.ops):
            if op["dma_sem"] is not None:
                dmas.append(i)
            else:
                last[op["eng"]] = i
        deps = set(last.values()) | set(self.last_dma_on_sem.values())
        for e in self.ENGS:
            self.pending_barrier[e] = set(deps)

    def emit(self, block_engines, sems, dma_sems):
        ops = self.ops
        n = len(ops)
        has_consumer = [False] * n
        for i, op in enumerate(ops):
            keep = set()
            for d in op["deps"]:
                dop = ops[d]
                if (dop["dma_sem"] is None and op["dma_sem"] is None
                        and dop["eng"] == "pe" and op["eng"] == "pe"):
                    continue
                keep.add(d)
                has_consumer[d] = True
            op["deps"] = keep
        sig = {e: 0 for e in self.ENGS}
        for i, op in enumerate(ops):
            if op["dma_sem"] is None and has_consumer[i]:
                assert op["fn"] is not None
                sig[op["eng"]] += 1
                op["sig"] = sig[op["eng"]]
            else:
                op["sig"] = None
        per_eng = {e: [] for e in self.ENGS}
        for i, op in enumerate(ops):
            per_eng[op["eng"]].append(i)

        def make_body(e):
            def body(eng):
                waited = {}
                for i in per_eng[e]:
                    op = ops[i]
                    need = {}
                    for d in op["deps"]:
                        dop = ops[d]
                        if dop["dma_sem"] is not None:
                            key = ("d", dop["dma_sem"])
                            val = dop["dma_val"]
                        else:
                            key = ("e", dop["eng"])
                            val = dop["sig"]
                        if val > need.get(key, 0):
                            need[key] = val
                    for key, val in need.items():
                        if val > waited.get(key, 0):
                            waited[key] = val
                            s = dma_sems[key[1]] if key[0] == "d" else sems[key[1]]
                            eng.wait_ge(s, val)
                    if op["fn"] is None:
                        continue
                    ins = op["fn"](eng)
                    if op["dma_sem"] is not None:
                        ins.then_inc(dma_sems[op["dma_sem"]], 16)
                    elif op["sig"] is not None:
                        ins.then_inc(sems[e], 1)
            return body

        for e in self.ENGS:
            if per_eng[e]:
                block_engines[e](make_body(e))


SB_BASE = 16512
SB_TOP = 229344
_cnt = [0]


class Arena:
    def __init__(self, nc, start=SB_BASE):
        self.nc = nc
        self.base = start
        self.top = SB_TOP
        self.cur = start
        self.peak = 0

    def alloc(self, name, shape, dt):
        esz = 2 if dt == BF16 else 4
        size = esz
        for s in shape[1:]:
            size *= s
        size = (size + 31) // 32 * 32
        assert self.cur + size <= self.top, f"SBUF overflow allocating {name}: {self.cur + size - self.base}"
        t = self.nc.alloc_sbuf_tensor_at(f"{name}_{_cnt[0]}", list(shape), dt, offset=self.cur)
        _cnt[0] += 1
        self.cur += size
        self.peak = max(self.peak, self.cur - self.base)
        return t

    def mark(self):
        return self.cur

    def release(self, m):
        self.cur = m


def host_consts():
    c = np.zeros((128, 784), np.float32)
    c[:, 0:128] = np.eye(128, dtype=np.float32)
    s = np.arange(128)[:, None]
    t = np.arange(128)[None, :]
    c[:, 128:256] = ((s // 64 == t // 64) & (s <= t)).astype(np.float32)
    inv_freq = (10000.0 ** (-np.arange(0, 32, 2, dtype=np.float32) / np.float32(32))).astype(np.float32)
    c[96:112, 256] = inv_freq
    c[112:128, 256] = inv_freq
    c[:, 272:400] = (s < t).astype(np.float32)
    c[:, 400:784] = np.arange(384, dtype=np.float32)[None, :]
    return c


def build_program(debug=(), n_experts=NE, stop_after=None):
    nc = bass.Bass("TRN2", target_bir_lowering=False)
    dr = {}

    def din(name, shape, dt=F32):
        dr[name] = nc.dram_tensor(name, list(shape), dt, kind="ExternalInput").ap()
        return dr[name]

    x = din("x", [S, D])
    pos = din("pos", [1, S], I32)
    cst = din("cst", [128, 784])
    ln_in_g = din("ln_in_g", [1, D]); ln_in_b = din("ln_in_b", [1, D])
    w_in = din("w_in", [D, INC])
    q_a_norm_g = din("q_a_norm_g", [3, 128])
    w_q_b = din("w_q_b", [384, 768])
    kv_a_norm_g = din("kv_a_norm_g", [2, 128])
    w_kv_b = din("w_kv_b", [256, 1024])
    lb_logits = din("hgrn_lb_logits", [8, 128])
    mla_out_g = din("mla_out_g", [4, 128])
    hgrn_out_g = din("hgrn_out_g", [4, 128])
    w_o = din("w_o", [D, D])
    ln1_g = din("ln1_g", [1, D]); ln1_b = din("ln1_b", [1, D])
    w_router = din("w_router", [D, NE])
    b_router = din("b_router", [1, NE])
    w_up = din("w_up", [NE, D, 2048])
    b_up = din("b_up", [NE * 16, 128])
    w_down = din("w_down", [NE, D, D])
    b_down = din("b_down", [NE, D])
    ln2_g = din("ln2_g", [1, D]); ln2_b = din("ln2_b", [1, D])
    out = nc.dram_tensor("out", [S, D], F32, kind="ExternalOutput").ap()
    h1d = nc.dram_tensor("h1_scratch", [S, D], F32).ap()

    sc = Sched()
    ar = Arena(nc)
    ps = nc.alloc_psum_tensor("ps", [128, 8, 512], F32)

    dma_names = []

    def A(eng, fn, r=(), w=(), dma=None):
        if dma is not None and dma not in dma_names:
            dma_names.append(dma)
        return sc.add(eng, fn, reads=r, writes=w, dma_sem=dma)

    taps = {}

    def tap(name, ap_fn, shape, dt, reads):
        if name not in debug:
            return
        t = nc.dram_tensor("dbg_" + name, list(shape), dt, kind="ExternalOutput").ap()
        taps[name] = t
        A("sp", lambda e: e.dma_start(out=t, in_=ap_fn()), r=reads, w=[("tapout", name)], dma="tap_" + name)

    def psb(b):
        return ps[:, b, :]

    def psb16(b):
        return ps[:, b, :].bitcast(BF16)

    cs = ar.alloc("cs", [128, 784], F32)
    identf = cs[:, 0:128]
    m2 = cs[:, 128:256]
    invf = cs[:, 256:257]
    identb = ar.alloc("identb", [128, 128], BF16)
    onesb = ar.alloc("onesb", [128, 128], BF16)
    oz = ar.alloc("oz", [128, 192], BF16)
    pst = ar.alloc("pst", [128, 128], F32)
    pcol = ar.alloc("pcol", [128, 32], F32)
    lbc = ar.alloc("lbc", [128, 4], F32)
    omlb = ar.alloc("omlb", [128, 4], F32)
    stat_rs = ar.alloc("stat_rs", [128, NT], F32)
    stat_nm = ar.alloc("stat_nm", [128, NT], F32)
    MIX_AT = ar.cur
    mixT = ar.alloc("mixT", [128, 8, S], BF16)
    P0_END = ar.cur
    Wq = ar.alloc("Wq", [128, 3, 8, 128], BF16)
    Wq2 = ar.alloc("Wq2", [128, 3, 8, 128], BF16)
    Wk = ar.alloc("Wk", [128, 2, 8, 64], BF16)
    Wv = ar.alloc("Wv", [128, 2, 8, 64], BF16)
    CW_END = ar.cur

    A("sp", lambda e: e.dma_start(out=cs[:], in_=cst), w=["cs"], dma="c0")
    A("dve", lambda e: e.tensor_copy(out=identb[:], in_=identf), r=["cs"], w=["identb"])
    A("dve", lambda e: e.memset(onesb[:], 1.0), w=["onesb"])
    A("dve", lambda e: e.memset(oz[:], 0.0), w=["oz"])
    A("dve", lambda e: e.memset(oz[:, 64:128], 1.0), w=["oz"])
    A("dve", lambda e: e.memset(pst[:], 0.0), w=["pst"])
    A("sp", lambda e: e.dma_start(out=pst[0:3, :], in_=q_a_norm_g), w=["pst"], dma="c1")
    A("sp", lambda e: e.dma_start(out=pst[3:5, :], in_=kv_a_norm_g), w=["pst"], dma="c1")
    A("sp", lambda e: e.dma_start(out=pst[5:9, :], in_=mla_out_g), w=["pst"], dma="c1")
    A("sp", lambda e: e.dma_start(out=pst[9:13, :], in_=hgrn_out_g), w=["pst"], dma="c1")
    A("sp", lambda e: e.dma_start(out=pst[13:21, :], in_=lb_logits), w=["pst"], dma="c1")
    A("pe", lambda e: e.transpose(out=ps[:, 0, 0:128], in_=pst[:], identity=identf), r=["pst", "cs"], w=[("ps", 0)])
    A("dve", lambda e: e.tensor_copy(out=pcol[:], in_=ps[:, 0, 0:32]), r=[("ps", 0)], w=["pcol"])
    A("dve", lambda e: e.tensor_tensor(out=omlb[:], in0=pcol[:, 13:17], in1=pcol[:, 17:21], op=ALU.subtract), r=["pcol"], w=["omlb"])
    A("act", lambda e: e.activation(out=lbc[:], in_=omlb[:], func=AF.Sigmoid), r=["omlb"], w=["lbc"])
    A("dve", lambda e: e.tensor_scalar(out=omlb[:], in0=lbc[:], scalar1=-1.0, scalar2=1.0, op0=ALU.mult, op1=ALU.add), r=["lbc"], w=["omlb"])
    QG, KVG, MOG, HOG = 0, 3, 5, 9

    def body():
        nonlocal ar
        hT = ar.alloc("hT", [128, 8, S], BF16)
        Win = ar.alloc("Win", [128, 8, INC], BF16)
        Wkr2 = ar.alloc("Wkr2", [128, 8, 128], BF16)
        Y_START = ar.cur
        ar = Arena(nc, Y_START)
        Gb = ar.alloc("Gb", [128, D], F32)
        Bb = ar.alloc("Bb", [128, D], F32)
        A("sp", lambda e: e.dma_start(out=Gb[:], in_=ln_in_g.partition_broadcast(128)), w=["Gb"], dma="c2")
        A("sp", lambda e: e.dma_start(out=Bb[:], in_=ln_in_b.partition_broadcast(128)), w=["Bb"], dma="c3")

        xt = ar.alloc("xt", [128, 2, D], F32)
        xn = ar.alloc("xn", [128, 2, D], F32)
        xg = ar.alloc("xg", [128, 2, D], F32)
        hb = ar.alloc("hb", [128, 2, D], BF16)
        stt = ar.alloc("stt", [128, 2, 2, 6], F32)
        mv = ar.alloc("mv", [128, 2, 2], F32)
        sd = ar.alloc("sd", [128, 2, 1], F32)
        wst = ar.alloc("wst", [128, 2, INC], F32)
        wsq = ar.alloc("wsq", [128, 3, 768], F32)
        wskv = ar.alloc("wskv", [128, 2, 1024], F32)
        A("sp", lambda e: e.dma_start(out=wsq[:], in_=w_q_b.rearrange("(kc p) n -> p kc n", p=128)), w=["wsq"], dma="c5")
        A("sp", lambda e: e.dma_start(out=wskv[:], in_=w_kv_b.rearrange("(kc p) n -> p kc n", p=128)), w=["wskv"], dma="c6")
        A("pool", lambda e: e.memset(Wq2[:], 0.0), w=["Wq2"])
        A("pool", lambda e: e.memset(Wq[:], 0.0), w=["Wq"])
        for kc in range(3):
            wv_ = lambda kc=kc: wsq[:, kc, :].rearrange("p (h c) -> p h c", c=96)
            A("dve", lambda e, kc=kc, wv_=wv_: e.tensor_scalar(out=Wq[:, kc, :, 0:64], in0=wv_()[:, :, 0:64], scalar1=pcol[:, QG + kc:QG + kc + 1], scalar2=None, op0=ALU.mult), r=["wsq", "pcol"], w=["Wq"])
            A("dve", lambda e, kc=kc, wv_=wv_: e.tensor_scalar(out=Wq[:, kc, :, 96:128], in0=wv_()[:, :, 64:96], scalar1=pcol[:, QG + kc:QG + kc + 1], scalar2=None, op0=ALU.mult), r=["wsq", "pcol"], w=["Wq"])
            A("pool", lambda e, kc=kc: e.tensor_scalar(out=Wq2[:, kc, :, 96:112], in0=Wq[:, kc, :, 112:128], scalar1=-1.0, scalar2=None, op0=ALU.mult), r=["Wq"], w=["Wq2"])
            A("pool", lambda e, kc=kc: e.tensor_copy(out=Wq2[:, kc, :, 112:128], in_=Wq[:, kc, :, 96:112]), r=["Wq"], w=["Wq2"])
        for kc in range(2):
            A("dve", lambda e, kc=kc: e.tensor_scalar(out=Wk[:, kc, :, :], in0=wskv[:, kc, :].rearrange("p (h c) -> p h c", c=128)[:, :, 0:64], scalar1=pcol[:, KVG + kc:KVG + kc + 1], scalar2=None, op0=ALU.mult), r=["wskv", "pcol"], w=["Wk"])
            A("dve", lambda e, kc=kc: e.tensor_scalar(out=Wv[:, kc, :, :], in0=wskv[:, kc, :].rearrange("p (h c) -> p h c", c=128)[:, :, 64:128], scalar1=pcol[:, KVG + kc:KVG + kc + 1], scalar2=None, op0=ALU.mult), r=["wskv", "pcol"], w=["Wv"])

        def ln_stats(src_fn, s, key_src, rs_ap, nm_ap, key_out, eps=1e-5):
            for hh in range(2):
                A("dve", lambda e, hh=hh: e.bn_stats(out=stt[:, s, hh, :], in_=src_fn()[:, hh * 512:(hh + 1) * 512]), r=[key_src], w=[("stt", s, hh)])
            A("dve", lambda e: e.bn_aggr(out=mv[:, s, :], in_=stt[:, s, :, :]), r=[("stt", s, 0), ("stt", s, 1)], w=[("mv", s)])
            A("act", lambda e: e.activation(out=sd[:, s, :], in_=mv[:, s, 1:2], func=AF.Sqrt, bias=eps, scale=1.0), r=[("mv", s)], w=[("sd", s)])
            A("dve", lambda e: e.reciprocal(out=rs_ap, in_=sd[:, s, :]), r=[("sd", s)], w=[key_out + ("rs",)])
            A("dve", lambda e: e.scalar_tensor_tensor(out=nm_ap, in0=mv[:, s, 0:1], scalar=-1.0, in1=rs_ap, op0=ALU.mult, op1=ALU.mult), r=[("mv", s), key_out + ("rs",)], w=[key_out + ("nm",)])

        def load_win(kc):
            s = kc % 2
            A("sp", lambda e: e.dma_start(out=wst[:, s, :], in_=w_in[kc * 128:(kc + 1) * 128, :]), w=[("wst", s)], dma=f"wst{s}")
            A("act", lambda e: e.activation(out=Win[:, kc, :], in_=wst[:, s, :], func=AF.Copy), r=[("wst", s)], w=[("Win", kc)])

        for i in range(NT):
            s = i % 2
            A("sp", lambda e, i=i, s=s: e.dma_start(out=xt[:, s, :], in_=x[i * 128:(i + 1) * 128, :]), w=[("xt", s)], dma=f"x{s}")
            if i < 8:
                load_win(i)
            ln_stats(lambda s=s: xt[:, s, :], s, ("xt", s), stat_rs[:, i:i + 1], stat_nm[:, i:i + 1], ("st", i))
            A("act", lambda e, s=s, i=i: e.activation(out=xn[:, s, :], in_=xt[:, s, :], func=AF.Identity, bias=stat_nm[:, i:i + 1], scale=stat_rs[:, i:i + 1]), r=[("xt", s), ("st", i, "rs"), ("st", i, "nm")], w=[("xn", s)])
            A("dve", lambda e, s=s: e.tensor_tensor(out=xg[:, s, :], in0=xn[:, s, :], in1=Gb[:], op=ALU.mult), r=[("xn", s), "Gb"], w=[("xg", s)])
            A("dve", lambda e, s=s: e.tensor_tensor(out=hb[:, s, :], in0=xg[:, s, :], in1=Bb[:], op=ALU.add), r=[("xg", s), "Bb"], w=[("hb", s)])
            for kc in range(8):
                A("pe", lambda e, s=s, kc=kc: e.transpose(out=psb16(s)[:, kc * 128:(kc + 1) * 128], in_=hb[:, s, kc * 128:(kc + 1) * 128], identity=identb[:]), r=[("hb", s), "identb"], w=[("ps", s)])
            A("act", lambda e, s=s, i=i: e.activation(out=hT[:, :, i * 128:(i + 1) * 128], in_=psb16(s).rearrange("p (k t) -> p k t", k=8), func=AF.Copy), r=[("ps", s)], w=[("hT", i // 4)])
        tap("hT", lambda: hT[:, 0, :], [128, S], BF16, [("hT", g) for g in range(4)])
        A("dve", lambda e: e.memset(Wkr2[:], 0.0), w=["Wkr2"])
        A("dve", lambda e: e.tensor_scalar(out=Wkr2[:, :, 96:112], in0=Win[:, :, 656:672], scalar1=-1.0, scalar2=None, op0=ALU.mult), r=[("Win", k) for k in range(8)], w=["Wkr2"])
        A("dve", lambda e: e.tensor_copy(out=Wkr2[:, :, 112:128], in_=Win[:, :, 640:656]), r=[("Win", k) for k in range(8)], w=["Wkr2"])
        WinK = [("Win", k) for k in range(8)]
        sc.barrier()
        LN_PEAK = ar.cur
        if stop_after == 'ln':
            return

        ar = Arena(nc, Y_START)
        scanmask = ar.alloc("scanmask", [128, S], F32)
        A("pool", lambda e: e.memset(scanmask[:], 1.0), w=["scanmask"])
        A("pool", lambda e: e.memset(scanmask[:, 0:S:64], 0.0), w=["scanmask"])
        qsT = ar.alloc("qsT", [128, S], BF16)
        gsT = ar.alloc("gsT", [128, S], BF16)
        fT = ar.alloc("fT", [128, S], F32)
        lfT = ar.alloc("lfT", [128, S], F32)
        bT = ar.alloc("bT", [128, S], F32)
        ebT = ar.alloc("ebT", [128, S], F32)
        qbT = qsT
        qbK = [("qsT", g) for g in range(4)]
        kbT = ar.alloc("kbT", [128, S], BF16)
        vtok = ar.alloc("vtok", [128, NT, 128], BF16)
        kbtok = ar.alloc("kbtok", [128, NT, 128], BF16)
        dcol = ar.alloc("dcol", [128, 32], F32)
        sig = ar.alloc("sig", [128, 2, 512], F32)
        scm = ar.alloc("scm", [128, 2, 128], BF16)
        Tst = ar.alloc("Tst", [128, 2, 128], F32)
        stbf = ar.alloc("stbf", [128, 2, 128], BF16)
        osq = ar.alloc("osq", [128, 512], BF16)
        lnv = ar.alloc("lnv", [128, 512], F32)
        Rv = ar.alloc("Rv", [128, 512], F32)
        ot = ar.alloc("ot", [128, 512], F32)
        for h in range(4):
            cq, cf, ci, cg = 672 + h * 128, 1184 + h * 128, 1696 + h * 128, 2208 + h * 128
            n = 0
            for tg in range(4):
                tsl = slice(tg * 512, (tg + 1) * 512)
                for which, c0 in (("q", cq), ("g", cg), ("f", cf)):
                    b = n % 2
                    n += 1
                    for kc in range(8):
                        A("pe", lambda e, b=b, kc=kc, c0=c0, tsl=tsl: e.matmul(psb(b), lhsT=Win[:, kc, c0:c0 + 128], rhs=hT[:, kc, tsl], start=(kc == 0), stop=(kc == 7)), r=WinK + [("hT", tg)], w=[("ps", b)])
                    if which == "q":
                        A("act", lambda e, b=b, tsl=tsl: e.activation(out=qsT[:, tsl], in_=psb(b), func=AF.Silu), r=[("ps", b)], w=[("qsT", tg)])
                    elif which == "g":
                        A("act", lambda e, b=b, tsl=tsl: e.activation(out=gsT[:, tsl], in_=psb(b), func=AF.Silu), r=[("ps", b)], w=[("gsT", tg)])
                    else:
                        A("act", lambda e, b=b: e.activation(out=sig[:, b, :], in_=psb(b), func=AF.Sigmoid), r=[("ps", b)], w=[("sig", b)])
                        A("dve", lambda e, b=b, tsl=tsl, h=h: e.tensor_scalar(out=fT[:, tsl], in0=sig[:, b, :], scalar1=omlb[:, h:h + 1], scalar2=lbc[:, h:h + 1], op0=ALU.mult, op1=ALU.add), r=[("sig", b), "omlb", "lbc"], w=[("fT", tg)])
            fTk = [("fT", g) for g in range(4)]
            for i in range(NT):
                if i % 4 == 0:
                    pass
                for kc in range(8):
                    A("pe", lambda e, i=i, kc=kc, ci=ci: e.matmul(ps[:, 2, (i % 4) * 128:(i % 4 + 1) * 128], lhsT=hT[:, kc, i * 128:(i + 1) * 128], rhs=Win[:, kc, ci:ci + 128], start=(kc == 0), stop=(kc == 7)), r=WinK + [("hT", i // 4)], w=[("ps", 2)])
                if i % 4 == 3:
                    A("dve", lambda e, i=i: e.tensor_copy(out=vtok[:, i - 3:i + 1, :], in_=psb(2).rearrange("p (a b) -> p a b", a=4)), r=[("ps", 2)], w=["vtok"])
            A("act", lambda e: e.activation(out=lfT[:], in_=fT[:], func=AF.Ln), r=fTk, w=["lfT"])
            A("dve", lambda e: e.tensor_tensor_scan(out=bT[:], data0=scanmask[:], data1=lfT[:], initial=0.0, op0=ALU.mult, op1=ALU.add), r=["scanmask", "lfT"], w=["bT"])
            A("act", lambda e: e.activation(out=ebT[:], in_=bT[:], func=AF.Exp), r=["bT"], w=["ebT"])
            A("act", lambda e: e.activation(out=lfT[:], in_=bT[:], func=AF.Exp, scale=-1.0), r=["bT"], w=["lfT"])
            A("act", lambda e: e.activation(out=fT[:], in_=fT[:], func=AF.Identity, bias=1.0, scale=-1.0), r=fTk, w=fTk)
            A("dve", lambda e: e.tensor_tensor(out=qbT[:], in0=qsT[:], in1=ebT[:], op=ALU.mult), r=[("qsT", g) for g in range(4)] + ["ebT"], w=qbK)
            A("dve", lambda e: e.tensor_tensor(out=kbT[:], in0=fT[:], in1=lfT[:], op=ALU.mult), r=fTk + ["lfT"], w=["kbT"])
            A("dve", lambda e: e.tensor_copy(out=dcol[:], in_=ebT[:, 63:S:64]), r=["ebT"], w=["dcol"])
            for i in range(NT):
                A("pe", lambda e, i=i: e.transpose(out=psb16(2)[:, (i % 4) * 128:(i % 4 + 1) * 128], in_=kbT[:, i * 128:(i + 1) * 128], identity=identb[:]), r=["kbT", "identb"], w=[("ps", 2)])
                if i % 4 == 3:
                    A("act", lambda e, i=i: e.activation(out=kbtok[:, i - 3:i + 1, :], in_=psb16(2)[:, 0:512].rearrange("p (a b) -> p a b", a=4), func=AF.Copy), r=[("ps", 2)], w=["kbtok"])
            for i in range(NT):
                tsl = slice(i * 128, (i + 1) * 128)
                sb_ = i % 2
                A("pe", lambda e, tsl=tsl: e.matmul(ps[:, 3, 0:128], lhsT=kbT[:, tsl], rhs=qbT[:, tsl], start=True, stop=True), r=["kbT"] + qbK, w=[("ps", 3)])
                A("dve", lambda e, sb_=sb_: e.tensor_tensor(out=scm[:, sb_, :], in0=ps[:, 3, 0:128], in1=m2, op=ALU.mult), r=[("ps", 3), "cs"], w=[("scm", sb_)])
                for cc in range(2):
                    c = 2 * i + cc
                    pb_ = 4 + (c % 2)
                    lo = cc * 64
                    A("pe", lambda e, pb_=pb_, lo=lo, i=i: e.matmul(ps[:, pb_, 0:128], lhsT=kbtok[lo:lo + 64, i, :], rhs=vtok[lo:lo + 64, i, :], start=True, stop=True), r=["kbtok", "vtok"], w=[("ps", pb_)])
                oc = (i % 4) * 128
                A("pe", lambda e, i=i, sb_=sb_, oc=oc: e.matmul(ps[:, 6, oc:oc + 128], lhsT=vtok[:, i, :], rhs=scm[:, sb_, :], start=True, stop=False), r=["vtok", ("scm", sb_)], w=[("ps", 6)])
                for cc in range(2):
                    c = 2 * i + cc
                    pb_ = 4 + (c % 2)
                    tb = c % 2
                    if c > 0:
                        A("pe", lambda e, c=c, oc=oc, cc=cc: e.matmul(ps[:, 6, oc + cc * 64:oc + cc * 64 + 64], lhsT=stbf[:, (c - 1) % 2, :], rhs=qbT[:, c * 64:(c + 1) * 64], start=False, stop=(cc == 1)), r=[("stbf", (c - 1) % 2)] + qbK, w=[("ps", 6)])
                    if c == 0:
                        A("dve", lambda e, pb_=pb_, tb=tb: e.tensor_copy(out=Tst[:, tb, :], in_=ps[:, pb_, 0:128]), r=[("ps", pb_)], w=[("Tst", tb)])
                    else:
                        A("dve", lambda e, pb_=pb_, tb=tb, c=c: e.scalar_tensor_tensor(out=Tst[:, tb, :], in0=Tst[:, 1 - tb, :], scalar=dcol[:, c - 1:c], in1=ps[:, pb_, 0:128], op0=ALU.mult, op1=ALU.add), r=[("ps", pb_), ("Tst", 1 - tb), "dcol"], w=[("Tst", tb)])
                    if c < 31:
                        A("act", lambda e, tb=tb, c=c: e.activation(out=stbf[:, tb, :], in_=Tst[:, tb, :], func=AF.Identity, scale=dcol[:, c:c + 1]), r=[("Tst", tb), "dcol"], w=[("stbf", tb)])
                if i % 4 == 3:
                    tg = i // 4
                    tsl4 = slice(tg * 512, (tg + 1) * 512)
                    A("act", lambda e: e.activation(out=osq[:], in_=psb(6), func=AF.Square), r=[("ps", 6)], w=["osq"])
                    A("pe", lambda e: e.matmul(psb(7), lhsT=onesb[:], rhs=osq[:], start=True, stop=True), r=["onesb", "osq"], w=[("ps", 7)])
                    A("act", lambda e: e.activation(out=lnv[:], in_=psb(7), func=AF.Ln, bias=1e-6, scale=1.0 / 128), r=[("ps", 7)], w=["lnv"])
                    A("act", lambda e: e.activation(out=Rv[:], in_=lnv[:], func=AF.Exp, scale=-0.5), r=["lnv"], w=["Rv"])
                    A("dve", lambda e: e.tensor_tensor(out=ot[:], in0=psb(6), in1=Rv[:], op=ALU.mult), r=[("ps", 6), "Rv"], w=["ot"])
                    A("dve", lambda e, h=h, tsl4=tsl4: e.tensor_tensor(out=mixT[:, 4 + h, tsl4], in0=ot[:], in1=gsT[:, tsl4], op=ALU.mult), r=["ot", ("gsT", tg)], w=[("mixT", 4 + h, tg)])
        tap("mixh", lambda: mixT[:, 4:8, :], [128, 4, S], BF16, [("mixT", 4 + h, g) for h in range(4) for g in range(4)])
        sc.barrier()

        if stop_after == 'hgrn':
            return
        ar = Arena(nc, Y_START)
        qaT = ar.alloc("qaT", [128, 3, S], BF16)
        kvT = ar.alloc("kvT", [128, 2, S], BF16)
        cosT = ar.alloc("cosT", [128, S], F32)
        sinT = ar.alloc("sinT", [128, S], F32)
        KR = ar.alloc("KR", [128, S], BF16)
        MLA_END = ar.cur
        posi = ar.alloc("posi", [128, 512], I32)
        ang = ar.alloc("ang", [128, 512], F32)
        tk = ar.alloc("tk", [128, 512], F32)
        tki = ar.alloc("tki", [128, 512], I32)
        tr_ = ar.alloc("tr_", [128, 512], F32)
        sqq = ar.alloc("sqq", [128, 3, 512], BF16)
        qraw = ar.alloc("qraw", [128, 3, 512], F32)
        lnq = ar.alloc("lnq", [128, 512], F32)
        Rt = ar.alloc("Rt", [128, 512], F32)
        tr1 = ar.alloc("tr1", [128, 512], F32)
        tr2 = ar.alloc("tr2", [128, 512], F32)
        R = slice(96, 128)

        def sincos(dst, shift, key, tsl, tg):
            if shift != 0.0:
                A("pool", lambda e: e.tensor_scalar(out=tr_[R, :], in0=ang[R, :], scalar1=shift, scalar2=None, op0=ALU.add), r=["ang"], w=["tr_"])
            else:
                A("pool", lambda e: e.tensor_copy(out=tr_[R, :], in_=ang[R, :]), r=["ang"], w=["tr_"])
            A("dve", lambda e: e.tensor_scalar(out=tk[R, :], in0=tr_[R, :], scalar1=1.0 / TWO_PI, scalar2=None, op0=ALU.mult), r=["tr_"], w=["tk"])
            A("dve", lambda e: e.tensor_copy(out=tki[R, :], in_=tk[R, :]), r=["tk"], w=["tki"])
            A("dve", lambda e: e.tensor_copy(out=tk[R, :], in_=tki[R, :]), r=["tki"], w=["tk"])
            A("dve", lambda e: e.scalar_tensor_tensor(out=tr_[R, :], in0=tk[R, :], scalar=-C1, in1=tr_[R, :], op0=ALU.mult, op1=ALU.add), r=["tk", "tr_"], w=["tr_"])
            A("dve", lambda e: e.scalar_tensor_tensor(out=tr_[R, :], in0=tk[R, :], scalar=-C2, in1=tr_[R, :], op0=ALU.mult, op1=ALU.add), r=["tk", "tr_"], w=["tr_"])
            A("dve", lambda e: e.tensor_scalar(out=tk[R, :], in0=tr_[R, :], scalar1=PI, scalar2=-TWO_PI, op0=ALU.is_gt, op1=ALU.mult), r=["tr_"], w=["tk"])
            A("dve", lambda e: e.tensor_tensor(out=tr_[R, :], in0=tr_[R, :], in1=tk[R, :], op=ALU.add), r=["tr_", "tk"], w=["tr_"])
            A("dve", lambda e: e.tensor_scalar(out=tk[R, :], in0=tr_[R, :], scalar1=-PI, scalar2=TWO_PI, op0=ALU.is_lt, op1=ALU.mult), r=["tr_"], w=["tk"])
            A("dve", lambda e: e.tensor_tensor(out=tr_[R, :], in0=tr_[R, :], in1=tk[R, :], op=ALU.add), r=["tr_", "tk"], w=["tr_"])
            A("dve", lambda e: e.tensor_scalar(out=tr_[R, :], in0=tr_[R, :], scalar1=PI, scalar2=-PI, op0=ALU.min, op1=ALU.max), r=["tr_"], w=["tr_"])
            A("act", lambda e: e.activation(out=dst[R, tsl], in_=tr_[R, :], func=AF.Sin), r=["tr_"], w=[(key, tg)])

        for tg in range(4):
            tsl = slice(tg * 512, (tg + 1) * 512)
            A("sp", lambda e, tsl=tsl: e.dma_start(out=posi[R, :], in_=pos[:, tsl].partition_broadcast(32)), w=["posi"], dma="c4")
            A("dve", lambda e: e.tensor_copy(out=ang[R, :], in_=posi[R, :]), r=["posi"], w=["ang"])
            A("dve", lambda e: e.tensor_scalar(out=ang[R, :], in0=ang[R, :], scalar1=invf[R, :], scalar2=None, op0=ALU.mult), r=["ang", "cs"], w=["ang"])
            sincos(sinT, 0.0, "sinT", tsl, tg)
            sincos(cosT, PI / 2, "cosT", tsl, tg)
        cosK = [("cosT", g) for g in range(4)]
        sinK = [("sinT", g) for g in range(4)]
        tap("cos", lambda: cosT[96:112, :], [16, S], F32, cosK)
        tap("sin", lambda: sinT[96:112, :], [16, S], F32, sinK)
        if stop_after == 'rope':
            return

        n = 0
        for tg in range(4):
            tsl = slice(tg * 512, (tg + 1) * 512)
            for which, nj, c0, dstT, width in (("q", 3, 0, qaT, 384.0), ("kv", 2, 384, kvT, 256.0)):
                for j in range(nj):
                    b = n % 2
                    n += 1
                    for kc in range(8):
                        A("pe", lambda e, b=b, kc=kc, c=c0 + j * 128, tsl=tsl: e.matmul(psb(b), lhsT=Win[:, kc, c:c + 128], rhs=hT[:, kc, tsl], start=(kc == 0), stop=(kc == 7)), r=WinK + [("hT", tg)], w=[("ps", b)])
                    A("dve", lambda e, b=b, j=j: e.tensor_copy(out=qraw[:, j, :], in_=psb(b)), r=[("ps", b)], w=[("qraw", j)])
                    A("act", lambda e, b=b, j=j: e.activation(out=sqq[:, j, :], in_=psb(b), func=AF.Square), r=[("ps", b)], w=[("sqq", j)])
                for j in range(nj):
                    A("pe", lambda e, j=j, nj=nj: e.matmul(psb(2), lhsT=onesb[:], rhs=sqq[:, j, :], start=(j == 0), stop=(j == nj - 1)), r=["onesb", ("sqq", j)], w=[("ps", 2)])
                A("act", lambda e, width=width: e.activation(out=lnq[:], in_=psb(2), func=AF.Ln, bias=1e-6, scale=1.0 / width), r=[("ps", 2)], w=["lnq"])
                A("act", lambda e: e.activation(out=Rt[:], in_=lnq[:], func=AF.Exp, scale=-0.5), r=["lnq"], w=["Rt"])
                for j in range(nj):
                    A("dve", lambda e, j=j, dstT=dstT, tsl=tsl: e.tensor_tensor(out=dstT[:, j, tsl], in0=qraw[:, j, :], in1=Rt[:], op=ALU.mult), r=[("qraw", j), "Rt"], w=[(which + "T", tg)])
            if stop_after == 'mla_nokr':
                continue
            for kc in range(8):
                A("pe", lambda e, kc=kc, tsl=tsl: e.matmul(psb(4), lhsT=Win[:, kc, 544:672], rhs=hT[:, kc, tsl], start=(kc == 0), stop=(kc == 7)), r=WinK + [("hT", tg)], w=[("ps", 4)])
            for kc in range(8):
                A("pe", lambda e, kc=kc, tsl=tsl: e.matmul(psb(5), lhsT=Wkr2[:, kc, :], rhs=hT[:, kc, tsl], start=(kc == 0), stop=(kc == 7)), r=["Wkr2", ("hT", tg)], w=[("ps", 5)])
            A("dve", lambda e, tsl=tsl: e.tensor_tensor(out=tr1[R, :], in0=ps[R, 4, :], in1=cosT[R, tsl], op=ALU.mult), r=[("ps", 4), ("cosT", tg)], w=["tr1"])
            A("dve", lambda e, tsl=tsl: e.tensor_tensor(out=tr2[R, :], in0=ps[R, 5, :], in1=sinT[R, tsl], op=ALU.mult), r=[("ps", 5), ("sinT", tg)], w=["tr2"])
            A("pool", lambda e, tsl=tsl: e.tensor_tensor(out=KR[R, tsl], in0=tr1[R, :], in1=tr2[R, :], op=ALU.add), r=["tr1", "tr2"], w=[("KR", tg)])
        tap("qaT", lambda: qaT[:, 0, :], [128, S], BF16, [("qT", g) for g in range(4)])
        if stop_after == 'mla_nokr':
            return
        tap("KR", lambda: KR[96:128, :], [32, S], BF16, [("KR", g) for g in range(4)])
        sc.barrier()
        MLA_PEAK = ar.cur
        if stop_after == 'mla':
            return

        ar = Arena(nc, CW_END)
        QT = ar.alloc("QT", [128, 8, S], BF16)
        KT = ar.alloc("KT", [128, 8, S], BF16)
        t1 = ar.alloc("t1", [128, 2, 512], F32)
        t2 = ar.alloc("t2", [128, 2, 512], F32)
        assert ar.cur <= Y_START, (ar.cur, Y_START)
        ar = Arena(nc, MLA_END)
        Vz = ar.alloc("Vz", [128, NT, 17 * 64], BF16)
        A("dve", lambda e: e.memset(Vz[:], 0.0), w=["Vz"])
        A("dve", lambda e: e.memset(KT[64:96, :, :], 0.0), w=[("KT", h) for h in range(8)])
        for tg in range(4):
            tsl = slice(tg * 512, (tg + 1) * 512)
            for h in range(8):
                pb1, pb2, pbk = (h % 2) * 2, (h % 2) * 2 + 1, 4 + h % 2
                tb = h % 2
                for kc in range(3):
                    A("pe", lambda e, kc=kc, h=h, pb1=pb1, tsl=tsl: e.matmul(psb(pb1), lhsT=Wq[:, kc, h, :], rhs=qaT[:, kc, tsl], start=(kc == 0), stop=(kc == 2)), r=["Wq", ("qT", tg)], w=[("ps", pb1)])
                for kc in range(3):
                    A("pe", lambda e, kc=kc, h=h, pb2=pb2, tsl=tsl: e.matmul(psb(pb2), lhsT=Wq2[:, kc, h, :], rhs=qaT[:, kc, tsl], start=(kc == 0), stop=(kc == 2)), r=["Wq2", ("qT", tg)], w=[("ps", pb2)])
                for kc in range(2):
                    A("pe", lambda e, kc=kc, h=h, pbk=pbk, tsl=tsl: e.matmul(ps[0:64, pbk, :], lhsT=Wk[:, kc, h, :], rhs=kvT[:, kc, tsl], start=(kc == 0), stop=(kc == 1)), r=["Wk", ("kvT", tg)], w=[("ps", pbk)])
                A("act", lambda e, h=h, pb1=pb1, tsl=tsl: e.activation(out=QT[0:96, h, tsl], in_=ps[0:96, pb1, :], func=AF.Copy), r=[("ps", pb1)], w=[("QT", h)])
                A("dve", lambda e, pb1=pb1, tb=tb, tsl=tsl: e.tensor_tensor(out=t1[R, tb, :], in0=ps[R, pb1, :], in1=cosT[R, tsl], op=ALU.mult), r=[("ps", pb1), ("cosT", tg)], w=[("t1", tb)])
                A("dve", lambda e, pb2=pb2, tb=tb, tsl=tsl: e.tensor_tensor(out=t2[R, tb, :], in0=ps[R, pb2, :], in1=sinT[R, tsl], op=ALU.mult), r=[("ps", pb2), ("sinT", tg)], w=[("t2", tb)])
                A("dve", lambda e, h=h, tb=tb, tsl=tsl: e.tensor_tensor(out=QT[R, h, tsl], in0=t1[R, tb, :], in1=t2[R, tb, :], op=ALU.add), r=[("t1", tb), ("t2", tb)], w=[("QT", h)])
                A("act", lambda e, h=h, pbk=pbk, tsl=tsl: e.activation(out=KT[0:64, h, tsl], in_=ps[0:64, pbk, :], func=AF.Copy), r=[("ps", pbk)], w=[("KT", h)])
                A("act", lambda e, h=h, tsl=tsl: e.activation(out=KT[R, h, tsl], in_=KR[R, tsl], func=AF.Copy), r=[("KR", tg)], w=[("KT", h)])
        for i in range(NT):
            pbv = 6 + i % 2
            for kc in range(2):
                A("pe", lambda e, kc=kc, i=i, pbv=pbv: e.matmul(psb(pbv), lhsT=kvT[:, kc, i * 128:(i + 1) * 128], rhs=Wv[:, kc, :, :], start=(kc == 0), stop=(kc == 1)), r=["Wv", ("kvT", i // 4)], w=[("ps", pbv)])
            A("dve", lambda e, i=i, pbv=pbv: e.tensor_copy(out=Vz[:, i, 64:64 + 1024].rearrange("p (h c) -> p h c", c=128)[:, :, 0:64], in_=psb(pbv).rearrange("p (h c) -> p h c", c=64)), r=[("ps", pbv)], w=["Vz"])
        tap("QT0", lambda: QT[:, 0, :], [128, S], BF16, [("QT", 0)])
        tap("KT0", lambda: KT[:, 0, :], [128, S], BF16, [("KT", 0)])
        tap("V", lambda: Vz[:, 0, :], [128, 17 * 64], BF16, ["Vz"])
        sc.barrier()

        if stop_after == 'c':
            return
        ar = Arena(nc, Y_START)
        PT = [ar.alloc(f"PT{k}", [128, 512], BF16) for k in range(3)]
        Dinv = ar.alloc("Dinv", [128, 512], F32)
        attf = ar.alloc("attf", [128, 4, 512], F32)
        asq = ar.alloc("asq", [128, 2, 512], BF16)
        lnr = ar.alloc("lnr", [128, 512], F32)
        Ra = ar.alloc("Ra", [128, 512], F32)
        scale = 96.0 ** -0.5
        its = []
        it = 0
        for g in range(4):
            for pair in range(4):
                po_, pd_ = 3 + it % 2, 5 + it % 2
                it += 1
                nk = 4 * g + 4
                for kt in range(nk):
                    for hh in range(2):
                        its.append(dict(g=g, pair=pair, kt=kt, hh=hh, po=po_, pd=pd_, first=(kt == 0 and hh == 0), last=(kt == nk - 1 and hh == 1)))
        LA = 2
        deferred = []

        def emit_S(n):
            d = its[n]
            g, kt, h = d["g"], d["kt"], 2 * d["pair"] + d["hh"]
            q0 = max(g * 512, kt * 128)
            ncols = (g + 1) * 512 - q0
            sb_ = n % 3
            A("pe", lambda e: e.matmul(ps[:, sb_, 0:ncols], lhsT=KT[:, h, kt * 128:(kt + 1) * 128], rhs=QT[:, h, q0:q0 + ncols], start=True, stop=True), r=[("KT", h), ("QT", h)], w=[("ps", sb_)])
            A("act", lambda e: e.activation(out=PT[sb_][:, 0:ncols], in_=ps[:, sb_, 0:ncols], func=AF.Exp, scale=scale), r=[("ps", sb_)], w=[("PT", sb_)])
            if kt >= 4 * g:
                A("pool", lambda e: e.memset(PT[sb_][64:128, 0:64], 0.0), w=[("PT", sb_)])

        def emit_PV(n):
            d = its[n]
            g, kt, hh, pair = d["g"], d["kt"], d["hh"], d["pair"]
            h = 2 * pair + hh
            po_, pd_, first, last = d["po"], d["pd"], d["first"], d["last"]
            q0 = max(g * 512, kt * 128)
            ncols = (g + 1) * 512 - q0
            c0 = q0 - g * 512
            sb_ = n % 3
            vs = h * 128 + 64 if hh == 0 else h * 128
            os_ = 64 if hh == 0 else 0
            A("pe", lambda e: e.matmul(ps[:, po_, c0:c0 + ncols], lhsT=Vz[:, kt, vs:vs + 128], rhs=PT[sb_][:, 0:ncols], start=first, stop=last), r=["Vz", ("PT", sb_)], w=[("ps", po_)])
            A("pe", lambda e: e.matmul(ps[:, pd_, c0:c0 + ncols], lhsT=oz[:, os_:os_ + 128], rhs=PT[sb_][:, 0:ncols], start=first, stop=last), r=["oz", ("PT", sb_)], w=[("ps", pd_)])
            if last:
                A("dve", lambda e: e.reciprocal(out=Dinv[:], in_=psb(pd_)), r=[("ps", pd_)], w=["Dinv"])
                A("dve", lambda e: e.tensor_tensor(out=attf[:, pair, :], in0=psb(po_), in1=Dinv[:], op=ALU.mult), r=[("ps", po_), "Dinv"], w=[("attf", pair)])
                A("act", lambda e: e.activation(out=asq[:, pair % 2, :], in_=attf[:, pair, :], func=AF.Square), r=[("attf", pair)], w=[("asq", pair % 2)])

                def fin():
                    A("pe", lambda e: e.matmul(psb(7), lhsT=onesb[:], rhs=asq[:, pair % 2, :], start=(pair == 0), stop=(pair == 3)), r=["onesb", ("asq", pair % 2)], w=[("ps", 7)])
                    if pair == 3:
                        A("act", lambda e: e.activation(out=lnr[:], in_=psb(7), func=AF.Ln, bias=1e-6, scale=1.0 / 512), r=[("ps", 7)], w=["lnr"])
                        A("act", lambda e: e.activation(out=Ra[:], in_=lnr[:], func=AF.Exp, scale=-0.5), r=["lnr"], w=["Ra"])
                        for pp in range(4):
                            A("dve", lambda e, pp=pp: e.tensor_tensor(out=mixT[:, pp, g * 512:(g + 1) * 512], in0=attf[:, pp, :], in1=Ra[:], op=ALU.mult), r=[("attf", pp), "Ra"], w=[("mixT", pp, g)])
                deferred.append((n + 8, fin))

        for n in range(len(its) + LA):
            if n < len(its):
                emit_S(n)
            if n - LA >= 0:
                emit_PV(n - LA)
            while deferred and deferred[0][0] <= n - LA:
                deferred.pop(0)[1]()
        while deferred:
            deferred.pop(0)[1]()
        tap("mixa", lambda: mixT[:, 0:4, :], [128, 4, S], BF16, [("mixT", p_, g) for p_ in range(4) for g in range(4)])
        sc.barrier()

        if stop_after == 'attn':
            return
        ar = Arena(nc, P0_END)
        gate = ar.alloc("gate", [128, NT, NE], F32)
        gT = ar.alloc("gT", [32, S], F32)
        bucol = ar.alloc("bucol", [128, NE * 16], F32)
        Bd = ar.alloc("Bd", [32, D], F32)
        F_P_END = ar.cur
        Wo = ar.alloc("Wo", [128, 8, D], BF16)
        wos = ar.alloc("wos", [128, 2, D], F32)
        GbF = ar.alloc("Gb0", [128, D], F32); BbF = ar.alloc("Bb0", [128, D], F32)
        G1 = ar.alloc("G1", [128, D], F32); B1 = ar.alloc("B1", [128, D], F32)
        Wr = ar.alloc("Wr", [128, 8, NE], F32)
        brB = ar.alloc("brB", [128, NE], F32)
        xtF = ar.alloc("xt2", [128, 2, D], F32)
        xnF = ar.alloc("xn2", [128, 2, D], F32)
        hf_ = ar.alloc("hf_", [128, 2, D], F32)
        y_ = ar.alloc("y_", [128, 2, D], F32)
        h1 = ar.alloc("h1", [128, 2, D], F32)
        sttF = ar.alloc("stt2", [128, 2, 2, 6], F32)
        mvF = ar.alloc("mv2", [128, 2, 2], F32)
        sdF = ar.alloc("sd2", [128, 2, 1], F32)
        rs1 = ar.alloc("rs1", [128, 2, 1], F32)
        nm1 = ar.alloc("nm1", [128, 2, 1], F32)
        h1Tf = ar.alloc("h1Tf", [128, 8, 128], F32)
        lg = ar.alloc("lg", [128, 2, NE], F32)
        mx8 = ar.alloc("mx8", [128, 8], F32)
        nmx = ar.alloc("nmx", [128, 1], F32)
        ex = ar.alloc("ex", [128, NE], F32)
        msk = ar.alloc("msk", [128, NE], F32)
        ssum = ar.alloc("ssum", [128, 1], F32)
        bust = ar.alloc("bust", [128, 4, 128], F32)
        A("sp", lambda e: e.dma_start(out=GbF[:], in_=ln_in_g.partition_broadcast(128)), w=["Gb"], dma="c2")
        A("sp", lambda e: e.dma_start(out=BbF[:], in_=ln_in_b.partition_broadcast(128)), w=["Bb"], dma="c3")
        A("sp", lambda e: e.dma_start(out=G1[:], in_=ln1_g.partition_broadcast(128)), w=["G1"], dma="c4")
        A("sp", lambda e: e.dma_start(out=B1[:], in_=ln1_b.partition_broadcast(128)), w=["B1"], dma="c5")
        A("sp", lambda e: e.dma_start(out=Wr[:], in_=w_router.rearrange("(kc p) n -> p kc n", p=128)), w=["Wr"], dma="c6")
        A("sp", lambda e: e.dma_start(out=brB[:], in_=b_router.partition_broadcast(128)), w=["brB"], dma="c0")
        A("sp", lambda e: e.dma_start(out=Bd[:], in_=b_down), w=["Bd"], dma="c1")
        A("sp", lambda e: e.dma_start(out=bust[:], in_=b_up.rearrange("(a p) n -> p a n", p=128)), w=["bust"], dma="c7")
        for a in range(4):
            A("pe", lambda e, a=a: e.transpose(out=ps[:, 0, a * 128:(a + 1) * 128], in_=bust[:, a, :], identity=identf), r=["bust", "cs"], w=[("ps", 0)])
        A("dve", lambda e: e.tensor_copy(out=bucol[:], in_=psb(0)), r=[("ps", 0)], w=["bucol"])
        for kc in range(8):
            s = kc % 2
            A("sp", lambda e, kc=kc, s=s: e.dma_start(out=wos[:, s, :], in_=w_o[kc * 128:(kc + 1) * 128, :]), w=[("wos", s)], dma=f"wst{s}")
            gc = (MOG + kc) if kc < 4 else (HOG + kc - 4)
            A("dve", lambda e, kc=kc, s=s, gc=gc: e.tensor_scalar(out=Wo[:, kc, :], in0=wos[:, s, :], scalar1=pcol[:, gc:gc + 1], scalar2=None, op0=ALU.mult), r=[("wos", s), "pcol"], w=["Wo"])
        mixK = [("mixT", c, g) for c in range(8) for g in range(4)]
        def stA(i):
            s = i % 2
            A("sp", lambda e, i=i, s=s: e.dma_start(out=xtF[:, s, :], in_=x[i * 128:(i + 1) * 128, :]), w=[("xt", s)], dma=f"x{s}")
            A("act", lambda e, s=s, i=i: e.activation(out=xnF[:, s, :], in_=xtF[:, s, :], func=AF.Identity, bias=stat_nm[:, i:i + 1], scale=stat_rs[:, i:i + 1]), r=[("xt", s), ("st", i, "rs"), ("st", i, "nm")], w=[("xn", s)])
            A("dve", lambda e, s=s: e.tensor_tensor(out=xnF[:, s, :], in0=xnF[:, s, :], in1=GbF[:], op=ALU.mult), r=[("xn", s), "Gb"], w=[("xn", s)])
            A("dve", lambda e, s=s: e.tensor_tensor(out=hf_[:, s, :], in0=xnF[:, s, :], in1=BbF[:], op=ALU.add), r=[("xn", s), "Bb"], w=[("hf_", s)])
            for c in range(2):
                pb_ = 2 * s + c
                for kc in range(8):
                    A("pe", lambda e, i=i, kc=kc, c=c, pb_=pb_: e.matmul(psb(pb_), lhsT=mixT[:, kc, i * 128:(i + 1) * 128], rhs=Wo[:, kc, c * 512:(c + 1) * 512], start=(kc == 0), stop=(kc == 7)), r=mixK + ["Wo"], w=[("ps", pb_)])
                A("dve", lambda e, s=s, c=c, pb_=pb_: e.scalar_tensor_tensor(out=y_[:, s, c * 512:(c + 1) * 512], in0=hf_[:, s, c * 512:(c + 1) * 512], scalar=ALPHA, in1=psb(pb_), op0=ALU.mult, op1=ALU.add), r=[("hf_", s), ("ps", pb_)], w=[("y_", s, c)])
            for hh in range(2):
                A("dve", lambda e, s=s, hh=hh: e.bn_stats(out=sttF[:, s, hh, :], in_=y_[:, s, hh * 512:(hh + 1) * 512]), r=[("y_", s, hh)], w=[("stt", s, hh)])
            A("dve", lambda e, s=s: e.bn_aggr(out=mvF[:, s, :], in_=sttF[:, s, :, :]), r=[("stt", s, 0), ("stt", s, 1)], w=[("mv", s)])
            A("act", lambda e, s=s: e.activation(out=sdF[:, s, :], in_=mvF[:, s, 1:2], func=AF.Sqrt, bias=1e-5, scale=1.0), r=[("mv", s)], w=[("sd", s)])
            A("dve", lambda e, s=s: e.reciprocal(out=rs1[:, s, :], in_=sdF[:, s, :]), r=[("sd", s)], w=[("rs1", s)])
            A("dve", lambda e, s=s: e.scalar_tensor_tensor(out=nm1[:, s, :], in0=mvF[:, s, 0:1], scalar=-1.0, in1=rs1[:, s, :], op0=ALU.mult, op1=ALU.mult), r=[("mv", s), ("rs1", s)], w=[("nm1", s)])
            A("act", lambda e, s=s: e.activation(out=y_[:, s, :], in_=y_[:, s, :], func=AF.Identity, bias=nm1[:, s, :], scale=rs1[:, s, :]), r=[("y_", s, 0), ("y_", s, 1), ("rs1", s), ("nm1", s)], w=[("y_", s, 0), ("y_", s, 1)])
            A("dve", lambda e, s=s: e.tensor_tensor(out=y_[:, s, :], in0=y_[:, s, :], in1=G1[:], op=ALU.mult), r=[("y_", s, 0), ("y_", s, 1), "G1"], w=[("y_", s, 0), ("y_", s, 1)])
            A("dve", lambda e, s=s: e.tensor_tensor(out=h1[:, s, :], in0=y_[:, s, :], in1=B1[:], op=ALU.add), r=[("y_", s, 0), ("y_", s, 1), "B1"], w=[("h1", s)])
            A("sp", lambda e, i=i, s=s: e.dma_start(out=h1d[i * 128:(i + 1) * 128, :], in_=h1[:, s, :]), r=[("h1", s)], w=[("h1d", i)], dma=f"h1o{s}")

        def stB1(i):
            s = i % 2
            for kc in range(8):
                A("pe", lambda e, s=s, kc=kc: e.transpose(out=ps[:, 4 + kc // 4, (kc % 4) * 128:(kc % 4 + 1) * 128], in_=h1[:, s, kc * 128:(kc + 1) * 128], identity=identf), r=[("h1", s), "cs"], w=[("ps", 4 + kc // 4)])
            for c in range(2):
                A("act", lambda e, c=c: e.activation(out=h1Tf[:, c * 4:(c + 1) * 4, :], in_=psb(4 + c).rearrange("p (k t) -> p k t", k=4), func=AF.Copy), r=[("ps", 4 + c)], w=[("h1Tf", c)])
            for kc in range(8):
                A("pe", lambda e, kc=kc: e.matmul(ps[:, 6, 0:NE], lhsT=h1Tf[:, kc, :], rhs=Wr[:, kc, :], start=(kc == 0), stop=(kc == 7)), r=[("h1Tf", kc // 4), "Wr"], w=[("ps", 6)])
            A("dve", lambda e, s=s: e.tensor_tensor(out=lg[:, s, :], in0=ps[:, 6, 0:NE], in1=brB[:], op=ALU.add), r=[("ps", 6), "brB"], w=[("lg", s)])

        def stB2(i):
            s = i % 2
            A("dve", lambda e, s=s: e.max(out=mx8[:], in_=lg[:, s, :]), r=[("lg", s)], w=["mx8"])
            A("dve", lambda e: e.tensor_scalar(out=nmx[:], in0=mx8[:, 0:1], scalar1=-1.0, scalar2=None, op0=ALU.mult), r=["mx8"], w=["nmx"])
            A("act", lambda e, s=s: e.activation(out=ex[:], in_=lg[:, s, :], func=AF.Exp, bias=nmx[:], scale=1.0), r=[("lg", s), "nmx"], w=["ex"])
            A("dve", lambda e, s=s: e.tensor_scalar(out=msk[:], in0=lg[:, s, :], scalar1=mx8[:, 3:4], scalar2=None, op0=ALU.is_ge), r=[("lg", s), "mx8"], w=["msk"])
            A("dve", lambda e: e.tensor_tensor(out=ex[:], in0=ex[:], in1=msk[:], op=ALU.mult), r=["ex", "msk"], w=["ex"])
            A("dve", lambda e: e.reduce_sum(out=ssum[:], in_=ex[:], axis=mybir.AxisListType.X), r=["ex"], w=["ssum"])
            A("dve", lambda e: e.reciprocal(out=ssum[:], in_=ssum[:]), r=["ssum"], w=["ssum"])
            A("dve", lambda e, i=i: e.tensor_scalar(out=gate[:, i, :], in0=ex[:], scalar1=ssum[:], scalar2=None, op0=ALU.mult), r=["ex", "ssum"], w=[("gate", i)])
            A("pe", lambda e, i=i: e.transpose(out=ps[0:32, 7, 0:128], in_=gate[:, i, :], identity=identf), r=[("gate", i), "cs"], w=[("ps", 7)])
            A("act", lambda e, i=i: e.activation(out=gT[:, i * 128:(i + 1) * 128], in_=ps[0:32, 7, 0:128], func=AF.Copy), r=[("ps", 7)], w=[("gT", i)])

        for t in range(NT + 2):
            if t < NT:
                stA(t)
            if 0 <= t - 1 < NT:
                stB1(t - 1)
            if t - 2 >= 0:
                stB2(t - 2)

        tap("gate", lambda: gate[:], [128, NT, NE], F32, [("gate", i) for i in range(NT)])
        sc.barrier()
        ar = Arena(nc, F_P_END)

        if stop_after == 'f':
            return
        CAP = 384
        NB = CAP // 128
        Yacc = Arena(nc, MIX_AT).alloc("Yacc", [128, 8, D], F32)
        SCR0 = ar.cur
        actT = ar.alloc("actT", [128, 8, CAP], BF16)
        XgT = ar.alloc("XgT", [128, 8, CAP], BF16)
        Sel = ar.alloc("Sel", [128, 8, CAP], BF16)
        yo = Arena(nc, SCR0).alloc("yo", [128, 2, D], F32)
        SelT = ar.alloc("SelT", [128, NB, 1024], BF16)
        YSL_AT = ar.cur
        Yslot = ar.alloc("Yslot", [128, NB, D], BF16)
        h1b = ar.alloc("h1b", [128, 8, D], BF16)
        maskb = ar.alloc("maskb", [128, NT, NE], BF16)
        rankm = ar.alloc("rankm", [128, 8, NE], F32)
        strib = ar.alloc("strib", [128, 128], BF16)
        iotaC = cs[:, 400:400 + CAP]
        Wub = [ar.alloc(f"Wub{k}", [128, 8, 2048], BF16) for k in range(2)]
        Wdb = ar.alloc("Wdb", [128, 8, D], BF16)
        h1r = Arena(nc, YSL_AT).alloc("h1r", [128, 1, D], F32)
        bucol1 = bucol
        gate_s = gate
        gateK = [("gate", i) for i in range(NT)]
        A("dve", lambda e: e.tensor_scalar(out=bucol[:].rearrange("p (a b) -> p a b", b=16)[:, :, 8:16], in0=bucol[:].rearrange("p (a b) -> p a b", b=16)[:, :, 8:16], scalar1=1.0, scalar2=None, op0=ALU.add), r=["bucol"], w=["bucol", "bucol1"])
        A("dve", lambda e: e.tensor_scalar(out=maskb[:], in0=gate[:], scalar1=0.0, scalar2=None, op0=ALU.is_gt), r=gateK, w=["maskb"])
        A("dve", lambda e: e.tensor_scalar(out=gate_s[:], in0=gate[:], scalar1=1.0 / 1.702, scalar2=None, op0=ALU.mult), r=gateK, w=gateK + ["gate_s"])
        A("dve", lambda e: e.tensor_copy(out=strib[:], in_=cs[:, 272:400]), r=["cs"], w=["strib"])
        sc.barrier()
        gt_ = ar.alloc("gt_", [128, 1, CAP], F32)
        sg_ = ar.alloc("sg_", [128, 1, CAP], F32)
        lt_ = ar.alloc("lt_", [128, 1, CAP], F32)
        sttM = ar.alloc("stt3", [128, 2, 2, 6], F32)
        mvM = ar.alloc("mv3", [128, 2, 2], F32)
        sdM = ar.alloc("sd3", [128, 2, 1], F32)
        rs2 = ar.alloc("rs2", [128, 2, 1], F32)
        nm2 = ar.alloc("nm2", [128, 2, 1], F32)
        NSTG = min(6, (SB_TOP - ar.cur) // (D * 4))
        assert NSTG >= 3, NSTG
        stg = ar.alloc("stg", [128, NSTG, D], F32)
        G2 = stg[:, 0, :]
        B2 = stg[:, 1, :]
        nst = [0]

        def cast_copy(eng, out_fn, in_fn, r, w):
            if eng == "act":
                A("act", lambda e: e.activation(out=out_fn(), in_=in_fn(), func=AF.Copy), r=r, w=w)
            else:
                A(eng, lambda e: e.tensor_copy(out=out_fn(), in_=in_fn()), r=r, w=w)

        def load_up_piece(e_, wb, q):
            k = nst[0] % NSTG
            nst[0] += 1
            kc, ch = q // 2, q % 2
            A("sp", lambda e: e.dma_start(out=stg[:, k, :], in_=w_up[e_, kc * 128:(kc + 1) * 128, ch * 1024:(ch + 1) * 1024]), w=[("stg", k)], dma=f"stg{k}")
            cast_copy("dve" if nst[0] % 4 == 0 else "act", lambda: Wub[wb][:, kc, ch * 1024:(ch + 1) * 1024], lambda: stg[:, k, :], [("stg", k)], [("Wub", wb, kc)])

        def load_down_piece(e_, j):
            k = nst[0] % NSTG
            nst[0] += 1
            A("sp", lambda e: e.dma_start(out=stg[:, k, :], in_=w_down[e_, j * 128:(j + 1) * 128, :]), w=[("stg", k)], dma=f"stg{k}")
            cast_copy("dve" if nst[0] % 4 == 0 else "act", lambda: Wdb[:, j, :], lambda: stg[:, k, :], [("stg", k)], [("Wdb", j)])

        WdbK = [("Wdb", k) for k in range(8)]
        steps = [(half, e_) for half in range(2) for e_ in range(n_experts)]
        for q in range(16):
            load_up_piece(0, 0, q)
        for half in range(2):
            for il in range(8):
                i = half * 8 + il
                A("sp", lambda e, i=i: e.dma_start(out=h1r[:, 0, :], in_=h1d[i * 128:(i + 1) * 128, :]), r=[("h1d", i)], w=[("h1r", 0)], dma="x0")
                A("act", lambda e, il=il: e.activation(out=h1b[:, il, :], in_=h1r[:, 0, :], func=AF.Copy), r=[("h1r", 0)], w=[("h1b", il)])
                for c in range(2):
                    pb_ = 4 + 2 * (il % 2) + c
                    A("pe", lambda e, i=i, c=c, pb_=pb_: e.matmul(psb(pb_), lhsT=gT[:, i * 128:(i + 1) * 128], rhs=Bd[:, c * 512:(c + 1) * 512], start=True, stop=True), r=[("gT", i), "Bd"], w=[("ps", pb_)])
                    A("dve", lambda e, il=il, c=c, pb_=pb_: e.scalar_tensor_tensor(out=Yacc[:, il, c * 512:(c + 1) * 512], in0=h1r[:, 0, c * 512:(c + 1) * 512], scalar=ALPHA, in1=psb(pb_), op0=ALU.mult, op1=ALU.add), r=[("h1r", 0), ("ps", pb_)], w=[("Yacc", il, c)])
            sc.barrier()
            for il in range(8):
                for jl in range(il):
                    A("pe", lambda e, il=il, jl=jl, half=half: e.matmul(ps[:, 0, il * NE:(il + 1) * NE], lhsT=onesb[:], rhs=maskb[:, half * 8 + jl, :], start=(jl == 0), stop=False), r=["onesb", "maskb"], w=[("ps", 0)])
                A("pe", lambda e, il=il, half=half: e.matmul(ps[:, 0, il * NE:(il + 1) * NE], lhsT=strib[:], rhs=maskb[:, half * 8 + il, :], start=(il == 0), stop=True), r=["strib", "maskb"], w=[("ps", 0)])
            A("dve", lambda e, half=half: e.scalar_tensor_tensor(out=rankm[:], in0=maskb[:, half * 8:half * 8 + 8, :], scalar=-4096.0, in1=ps[:, 0, 0:8 * NE].rearrange("p (a b) -> p a b", a=8), op0=ALU.mult, op1=ALU.add), r=["maskb", ("ps", 0)], w=["rankm"])
            A("dve", lambda e: e.tensor_scalar(out=rankm[:], in0=rankm[:], scalar1=4096.0, scalar2=None, op0=ALU.add), r=["rankm"], w=["rankm"])
            def emit_sel(ee):
                for il in range(8):
                    A("dve", lambda e, il=il, ee=ee: e.tensor_scalar(out=Sel[:, il, :], in0=iotaC, scalar1=rankm[:, il, ee:ee + 1], scalar2=None, op0=ALU.is_equal), r=["rankm", "cs"], w=[("Sel", il)])

            for e_ in range(n_experts):
                st_i = half * n_experts + e_
                wb = st_i % 2
                nxt = steps[st_i + 1][1] if st_i + 1 < len(steps) else None
                WubK = [("Wub", wb, k) for k in range(8)]
                pend = [(nxt, 1 - wb, q) for q in range(16)] if nxt is not None else []

                def prefetch(n):
                    for _ in range(n):
                        if pend:
                            load_up_piece(*pend.pop(0))
                if e_ == 0:
                    emit_sel(0)
                for kc in range(8):
                    bk = kc % 2
                    order = [7, 6, 5, 4, 3, 2, 1, 0]
                    for oi, il in enumerate(order):
                        nsl = min(CAP, 128 * (il + 1))
                        A("pe", lambda e, bk=bk, kc=kc, il=il, nsl=nsl, oi=oi: e.matmul(ps[:, bk, 0:nsl], lhsT=h1b[:, il, kc * 128:(kc + 1) * 128], rhs=Sel[:, il, 0:nsl], start=(oi == 0), stop=(oi == 7)), r=[("h1b", il), ("Sel", il)], w=[("ps", bk)])
                    A("act", lambda e, bk=bk, kc=kc: e.activation(out=XgT[:, kc, :], in_=ps[:, bk, 0:CAP], func=AF.Copy), r=[("ps", bk)], w=[("XgT", kc)])
                    load_down_piece(e_, kc)
                XgK = [("XgT", k) for k in range(8)]
                for sb in range(NB):
                    for il in range(8):
                        A("pe", lambda e, sb=sb, il=il: e.transpose(out=psb16(4 + sb)[:, il * 128:(il + 1) * 128], in_=Sel[:, il, sb * 128:(sb + 1) * 128], identity=identb[:]), r=[("Sel", il), "identb"], w=[("ps", 4 + sb)])
                    A("act", lambda e, sb=sb: e.activation(out=SelT[:, sb, :], in_=psb16(4 + sb), func=AF.Copy), r=[("ps", 4 + sb)], w=[("SelT", sb)])
                for j in range(8):
                    bg_, bl_ = (2, 3) if j % 2 == 0 else (0, 1)
                    s = 0
                    for kc in range(8):
                        A("pe", lambda e, kc=kc, j=j, bg_=bg_, wb=wb: e.matmul(ps[:, bg_, 0:CAP], lhsT=Wub[wb][:, kc, j * 128:(j + 1) * 128], rhs=XgT[:, kc, :], start=(kc == 0), stop=(kc == 7)), r=WubK + XgK, w=[("ps", bg_)])
                    for kc in range(8):
                        A("pe", lambda e, kc=kc, j=j, bl_=bl_, wb=wb: e.matmul(ps[:, bl_, 0:CAP], lhsT=Wub[wb][:, kc, 1024 + j * 128:1024 + (j + 1) * 128], rhs=XgT[:, kc, :], start=(kc == 0), stop=(kc == 7)), r=WubK + XgK, w=[("ps", bl_)])
                    bg = e_ * 16 + j
                    bl = e_ * 16 + 8 + j
                    A("dve", lambda e, s=s, bg_=bg_, bg=bg: e.tensor_scalar(out=gt_[:, s, :], in0=ps[:, bg_, 0:CAP], scalar1=bucol[:, bg:bg + 1], scalar2=7.0, op0=ALU.add, op1=ALU.min), r=[("ps", bg_), "bucol"], w=[("gt_", s)])
                    A("act", lambda e, s=s: e.activation(out=sg_[:, s, :], in_=gt_[:, s, :], func=AF.Silu, scale=1.702), r=[("gt_", s)], w=[("sg_", s)])
                    A("dve", lambda e, s=s, bl_=bl_, bl=bl: e.tensor_scalar(out=lt_[:, s, :], in0=ps[:, bl_, 0:CAP], scalar1=bucol1[:, bl:bl + 1], scalar2=-6.0, op0=ALU.add, op1=ALU.max), r=[("ps", bl_), "bucol1"], w=[("lt_", s)])
                    A("dve", lambda e, s=s, j=j: e.scalar_tensor_tensor(out=actT[:, j, :], in0=lt_[:, s, :], scalar=8.0, in1=sg_[:, s, :], op0=ALU.min, op1=ALU.mult), r=[("sg_", s), ("lt_", s)], w=[("actT", j)])
                    prefetch(1)
                actK = [("actT", j) for j in range(8)]
                for sb in range(NB):
                    for c in range(2):
                        bk = 4 + (2 * sb + c) % 2
                        for j in range(8):
                            A("pe", lambda e, sb=sb, c=c, j=j, bk=bk: e.matmul(psb(bk), lhsT=actT[:, j, sb * 128:(sb + 1) * 128], rhs=Wdb[:, j, c * 512:(c + 1) * 512], start=(j == 0), stop=(j == 7)), r=actK + WdbK, w=[("ps", bk)])
                        A("act", lambda e, sb=sb, c=c, bk=bk: e.activation(out=Yslot[:, sb, c * 512:(c + 1) * 512], in_=psb(bk), func=AF.Copy), r=[("ps", bk)], w=[("Yslot", sb, c)])
                    prefetch(1)
                if e_ + 1 < n_experts:
                    emit_sel(e_ + 1)
                for il in range(8):
                    for c in range(2):
                        bk = 6 + (2 * il + c) % 2
                        nbl = min(NB, il + 1)
                        for sb in range(nbl):
                            A("pe", lambda e, il=il, c=c, sb=sb, bk=bk, nbl=nbl: e.matmul(psb(bk), lhsT=SelT[:, sb, il * 128:(il + 1) * 128], rhs=Yslot[:, sb, c * 512:(c + 1) * 512], start=(sb == 0), stop=(sb == nbl - 1)), r=[("SelT", sb), ("Yslot", sb, c)], w=[("ps", bk)])
                        A("dve", lambda e, il=il, c=c, bk=bk, half=half, e_=e_: e.scalar_tensor_tensor(out=Yacc[:, il, c * 512:(c + 1) * 512], in0=psb(bk), scalar=gate_s[:, half * 8 + il, e_:e_ + 1], in1=Yacc[:, il, c * 512:(c + 1) * 512], op0=ALU.mult, op1=ALU.add), r=[("ps", bk), "gate_s", ("Yacc", il, c)], w=[("Yacc", il, c)])
                    if il >= 3:
                        prefetch(1)
                prefetch(16)
            sc.barrier()
            A("sp", lambda e: e.dma_start(out=G2, in_=ln2_g.partition_broadcast(128)), w=["G2"], dma="c2")
            A("sp", lambda e: e.dma_start(out=B2, in_=ln2_b.partition_broadcast(128)), w=["B2"], dma="c3")
            for il in range(8):
                i = half * 8 + il
                s = il % 2
                for hh in range(2):
                    A("dve", lambda e, s=s, hh=hh, il=il: e.bn_stats(out=sttM[:, s, hh, :], in_=Yacc[:, il, hh * 512:(hh + 1) * 512]), r=[("Yacc", il, hh)], w=[("stt", s, hh)])
                A("dve", lambda e, s=s: e.bn_aggr(out=mvM[:, s, :], in_=sttM[:, s, :, :]), r=[("stt", s, 0), ("stt", s, 1)], w=[("mv", s)])
                A("act", lambda e, s=s: e.activation(out=sdM[:, s, :], in_=mvM[:, s, 1:2], func=AF.Sqrt, bias=1e-5, scale=1.0), r=[("mv", s)], w=[("sd", s)])
                A("dve", lambda e, s=s: e.reciprocal(out=rs2[:, s, :], in_=sdM[:, s, :]), r=[("sd", s)], w=[("rs2", s)])
                A("dve", lambda e, s=s: e.scalar_tensor_tensor(out=nm2[:, s, :], in0=mvM[:, s, 0:1], scalar=-1.0, in1=rs2[:, s, :], op0=ALU.mult, op1=ALU.mult), r=[("mv", s), ("rs2", s)], w=[("nm2", s)])
                A("act", lambda e, s=s, il=il: e.activation(out=yo[:, s, :], in_=Yacc[:, il, :], func=AF.Identity, bias=nm2[:, s, :], scale=rs2[:, s, :]), r=[("Yacc", il, 0), ("Yacc", il, 1), ("rs2", s), ("nm2", s)], w=[("yo", s)])
                A("dve", lambda e, s=s: e.tensor_tensor(out=yo[:, s, :], in0=yo[:, s, :], in1=G2, op=ALU.mult), r=[("yo", s), "G2"], w=[("yo", s)])
                A("dve", lambda e, s=s: e.tensor_tensor(out=yo[:, s, :], in0=yo[:, s, :], in1=B2, op=ALU.add), r=[("yo", s), "B2"], w=[("yo", s)])
                A("sp", lambda e, i=i, s=s: e.dma_start(out=out[i * 128:(i + 1) * 128, :], in_=yo[:, s, :]), r=[("yo", s)], w=[("out", i)], dma=f"o{s}")
            sc.barrier()

    body()
    A("sp", None, r=[("out", i) for i in range(NT)] + [("tapout", k) for k in taps])

    sems = {k: nc.alloc_semaphore("s_" + k) for k in Sched.ENGS}
    dsem = {k: nc.alloc_semaphore("d_" + k) for k in dma_names}
    with nc.Block() as block:
        sc.emit(dict(pe=block.tensor, act=block.scalar, dve=block.vector, pool=block.gpsimd, sp=block.sync), sems, dsem)
    return nc, taps, 0, len(sc.ops)


def core_inputs(inputs, b):
    f = np.float32
    g = lambda k: np.ascontiguousarray(inputs[k], dtype=f)
    return {
        "x": np.ascontiguousarray(inputs["x"][b], dtype=f),
        "pos": np.ascontiguousarray(inputs["positions"][b].reshape(1, S).astype(np.int32)),
        "cst": host_consts(),
        "ln_in_g": g("ln_in_g").reshape(1, D), "ln_in_b": g("ln_in_b").reshape(1, D),
        "w_in": g("w_in").reshape(D, INC),
        "q_a_norm_g": g("q_a_norm_g").reshape(3, 128),
        "w_q_b": g("w_q_b").reshape(384, 768),
        "kv_a_norm_g": g("kv_a_norm_g").reshape(2, 128),
        "w_kv_b": g("w_kv_b").reshape(256, 1024),
        "hgrn_lb_logits": g("hgrn_lb_logits").reshape(8, 128),
        "mla_out_g": g("mla_out_g").reshape(4, 128),
        "hgrn_out_g": g("hgrn_out_g").reshape(4, 128),
        "w_o": g("w_o").reshape(D, D),
        "ln1_g": g("ln1_g").reshape(1, D), "ln1_b": g("ln1_b").reshape(1, D),
        "w_router": g("w_router").reshape(D, NE),
        "b_router": g("b_router").reshape(1, NE),
        "w_up": g("w_up").reshape(NE, D, 2048),
        "b_up": g("b_up").reshape(NE * 16, 128),
        "w_down": g("w_down").reshape(NE, D, D),
        "b_down": g("b_down").reshape(NE, D),
        "ln2_g": g("ln2_g").reshape(1, D), "ln2_b": g("ln2_b").reshape(1, D),
    }


def kernel(**inputs):
    nc, _, _, _ = build_program()
    shared = core_inputs(inputs, 0)
    in_maps = []
    for b in range(8):
        m = dict(shared)
        m["x"] = np.ascontiguousarray(inputs["x"][b], dtype=np.float32)
        m["pos"] = np.ascontiguousarray(np.asarray(inputs["positions"][b]).reshape(1, S).astype(np.int32))
        in_maps.append(m)
    res = run_bass_kernel_spmd(nc, in_maps, core_ids=list(range(8)))
    return np.stack([np.asarray(r["out"], dtype=np.float32) for r in res.results], axis=0)
```
